# Optimizing a Trainium2 kernel written in Bass

```python
import math
import jax
import jax.numpy as jnp
from jax import lax
import numpy as np

D_MODEL = 2048
BATCH = 1
SEQ = 16384
DEPTH = 2

CTX_LEN = 256
GRID_W = 64
N_MOD = 6
EPS = 1e-6
CHUNK = 64
CONV_W = 4

DN_HEADS = D_MODEL // 256
DN_DK = 128
DN_DV = 128
DN_QK = DN_HEADS * DN_DK
DN_V = DN_HEADS * DN_DV
LRU_WIDTH = D_MODEL // 2
LRU_BLOCKS = 8
LRU_BW = LRU_WIDTH // LRU_BLOCKS
LRU_C = 8.0
EV_IN = 2 * DN_QK + 2 * DN_V + 4 * DN_HEADS + 2 * LRU_WIDTH
EV_MIX = DN_V + LRU_WIDTH
GLA_HEADS = 4
GLA_QK = D_MODEL // 2
GLA_V = D_MODEL
GLA_DK = GLA_QK // GLA_HEADS
GLA_DV = GLA_V // GLA_HEADS
GLA_RANK = 16
GLA_TAU = 16.0
OD_IN = 2 * GLA_QK + 2 * GLA_V + 2 * GLA_RANK
FFN_HIDDEN = 11 * D_MODEL // 4
N_EXPERTS = 8
TOP_K = 2
MOE_BLOCK = 128

N_EVEN = (DEPTH + 1) // 2
N_ODD = DEPTH // 2

kernel_name = 'hybrid_deltanet_rglru_gla_moe_dit'


def rms_norm(x, g):
    x32 = x.astype(jnp.float32)
    y = x32 * lax.rsqrt(jnp.mean(x32 * x32, axis=-1, keepdims=True) + EPS)
    return y.astype(x.dtype) * g


def l2_normalize(t):
    t32 = t.astype(jnp.float32)
    return (t32 * lax.rsqrt(jnp.sum(t32 * t32, axis=-1, keepdims=True) + EPS)).astype(t.dtype)


def adaln(cond, w, b):
    m = jax.nn.silu(cond) @ w + b
    m = m.reshape(m.shape[0], 1, N_MOD, -1)
    return tuple(m[:, :, j] for j in range(N_MOD))


def modulate(h, shift, scale):
    return h * (1.0 + scale) + shift


def split_cols(p, sizes):
    out, start = [], 0
    for s in sizes:
        out.append(p[..., start:start + s])
        start += s
    return out


def centred_conv(x, w):
    k_w, length = w.shape[0], x.shape[1]
    left = k_w // 2
    xp = jnp.pad(x, ((0, 0), (left, k_w - 1 - left), (0, 0)))
    out = xp[:, 0:length] * w[0]
    for j in range(1, k_w):
        out = out + xp[:, j:j + length] * w[j]
    return out


def raster_transpose(t, rows, cols):
    b, n, d = t.shape
    return t.reshape(b, rows, cols, d).swapaxes(1, 2).reshape(b, n, d)


def gated_delta_chunked(q, k, v, g, beta, s0):
    b_, h_, length, _ = q.shape
    dv = v.shape[-1]
    out_dtype = v.dtype
    n = length // CHUNK
    f32 = jnp.float32
    chunks = lambda t: t.astype(f32).reshape(b_, h_, n, CHUNK, *t.shape[3:])
    q, k, v, beta = chunks(q), chunks(k), chunks(v), chunks(beta)
    gc = jnp.cumsum(chunks(g), axis=-1)
    lower = jnp.tril(jnp.ones((CHUNK, CHUNK), bool))
    strict = jnp.tril(jnp.ones((CHUNK, CHUNK), bool), -1)
    decay = jnp.exp(jnp.where(lower, gc[..., :, None] - gc[..., None, :], -jnp.inf))
    kb = k * beta[..., None]
    a = jnp.where(strict, jnp.einsum('bhnid,bhnjd->bhnij', kb, k) * decay, 0.0)
    eye = jnp.eye(CHUNK, dtype=f32)
    t_inv = lax.linalg.triangular_solve(eye + a, jnp.broadcast_to(eye, a.shape), left_side=True, lower=True, unit_diagonal=True)
    u = t_inv @ (v * beta[..., None])
    w = t_inv @ (kb * jnp.exp(gc)[..., None])
    qk = jnp.einsum('bhnid,bhnjd->bhnij', q, k) * decay
    q_dec = q * jnp.exp(gc)[..., None]
    k_dec = k * jnp.exp(gc[..., -1:] - gc)[..., None]
    g_last = jnp.exp(gc[..., -1])

    def step(s, xs):
        u_c, w_c, q_c, qk_c, k_c, gl_c = xs
        v_new = u_c - w_c @ s
        o = q_c @ s + qk_c @ v_new
        s = s * gl_c[..., None, None] + jnp.swapaxes(k_c, -1, -2) @ v_new
        return s, o

    xs = tuple(jnp.moveaxis(t, 2, 0) for t in (u, w, q_dec, qk, k_dec, g_last))
    s, o = lax.scan(step, s0.astype(f32), xs)
    return jnp.moveaxis(o, 0, 2).reshape(b_, h_, length, dv).astype(out_dtype), s.astype(s0.dtype)


def gla_chunked(q, k, v, g, s0):
    b_, h_, length, _ = q.shape
    dv = v.shape[-1]
    out_dtype = v.dtype
    n = length // CHUNK
    f32 = jnp.float32
    chunks = lambda t: jnp.moveaxis(t.astype(f32).reshape(b_, h_, n, CHUNK, t.shape[-1]), 2, 0)
    gc = jnp.cumsum(chunks(g), axis=3)
    causal = jnp.tril(jnp.ones((CHUNK, CHUNK), bool))[:, :, None]

    def step(s, xs):
        q_c, k_c, v_c, g_c = xs
        rel = jnp.exp(jnp.where(causal, g_c[:, :, :, None] - g_c[:, :, None], -jnp.inf))
        att = jnp.einsum('bhid,bhjd,bhijd->bhij', q_c, k_c, rel)
        o = (q_c * jnp.exp(g_c)) @ s + att @ v_c
        s = s * jnp.exp(g_c[:, :, -1])[..., None] + jnp.swapaxes(k_c * jnp.exp(g_c[:, :, -1:] - g_c), -1, -2) @ v_c
        return s, o

    s, o = lax.scan(step, s0.astype(f32), (chunks(q), chunks(k), chunks(v), gc))
    return jnp.moveaxis(o, 0, 2).reshape(b_, h_, length, dv).astype(out_dtype), s.astype(s0.dtype)


def linear_recurrence(a, b, h0):
    b = b.at[:, 0].add(a[:, 0] * h0)

    def combine(l, r):
        return l[0] * r[0], r[0] * l[1] + r[1]

    _, h = lax.associative_scan(combine, (a, b), axis=1)
    return h, h[:, -1]


def prefix_scan(core, ctx_args, lat_args, s0, axis, reverse):
    if reverse:
        ctx_args = tuple(jnp.flip(t, axis) for t in ctx_args)
        lat_args = tuple(jnp.flip(t, axis) for t in lat_args)
    o_ctx, s_ctx = core(*ctx_args, s0)
    o_lat, _ = core(*lat_args, s_ctx)
    if reverse:
        o_ctx, o_lat = jnp.flip(o_ctx, axis), jnp.flip(o_lat, axis)
    return o_ctx, o_lat


def bidirectional(core, ctx_shared, ctx_dir, lat_shared, lat_dir, s0, axis):
    outs = []
    for d, rev in ((0, False), (1, True)):
        ca = tuple(ctx_shared) + tuple(t[d] for t in ctx_dir)
        la = tuple(lat_shared) + tuple(t[d] for t in lat_dir)
        outs.append(prefix_scan(core, ca, la, s0, axis, rev))
    (oc_f, ol_f), (oc_b, ol_b) = outs
    return oc_f + oc_b, ol_f + ol_b


def even_mixer(h_ctx, h_lat, w_in, conv_qkv, dn_a_log, dn_dt_bias, dn_norm_g, lru_conv_w, lru_conv_b,
               lru_wa, lru_ba, lru_wx, lru_bx, lru_lambda, w_out, ctx_out):
    f32 = jnp.float32

    def prepare(h):
        bn, ln, _ = h.shape
        qkv, z, a_raw, b_raw, xr, gr = split_cols(
            h @ w_in, (2 * DN_QK + DN_V, DN_V, 2 * DN_HEADS, 2 * DN_HEADS, LRU_WIDTH, LRU_WIDTH))
        q, k, v = split_cols(jax.nn.silu(centred_conv(qkv, conv_qkv)), (DN_QK, DN_QK, DN_V))
        heads = lambda t: t.reshape(bn, ln, DN_HEADS, -1).transpose(0, 2, 1, 3)
        q = l2_normalize(heads(q)) * DN_DK ** -0.5
        k = l2_normalize(heads(k))
        v = heads(v)
        a_raw = a_raw.reshape(bn, ln, 2, DN_HEADS).transpose(2, 0, 3, 1)
        b_raw = b_raw.reshape(bn, ln, 2, DN_HEADS).transpose(2, 0, 3, 1)
        g = -jnp.exp(dn_a_log)[:, None, :, None] * jax.nn.softplus(a_raw + dn_dt_bias[:, None, :, None])
        beta = jax.nn.sigmoid(b_raw)
        xc = centred_conv(xr, lru_conv_w) + lru_conv_b
        xb = xc.reshape(bn, ln, LRU_BLOCKS, LRU_BW)
        r = jax.nn.sigmoid(jnp.einsum('blnc,dncm->dblnm', xb, lru_wa).reshape(2, bn, ln, LRU_WIDTH) + lru_ba[:, None, None])
        gi = jax.nn.sigmoid(jnp.einsum('blnc,dncm->dblnm', xb, lru_wx).reshape(2, bn, ln, LRU_WIDTH) + lru_bx[:, None, None])
        log_a = -LRU_C * r.astype(f32) * jax.nn.softplus(-lru_lambda.astype(f32))[:, None, None]
        a = jnp.exp(log_a)
        b_in = jnp.sqrt(-jnp.expm1(2.0 * log_a)) * (gi * xc).astype(f32)
        return (q, k, v), (g, beta), (a, b_in), (z, gr)

    qkv_c, gb_c, ab_c, zg_c = prepare(h_ctx)
    qkv_l, gb_l, ab_l, zg_l = prepare(h_lat)
    bn = h_lat.shape[0]
    dn_ctx, dn_lat = bidirectional(gated_delta_chunked, qkv_c, gb_c, qkv_l, gb_l,
                                   jnp.zeros((bn, DN_HEADS, DN_DK, DN_DV), h_lat.dtype), 2)
    lru_ctx, lru_lat = bidirectional(linear_recurrence, (), ab_c, (), ab_l, jnp.zeros((bn, LRU_WIDTH), f32), 1)

    def finish(o_dn, h_lru, zg):
        z, gr = zg
        bn_, ln_ = z.shape[:2]
        o = rms_norm(o_dn.transpose(0, 2, 1, 3), dn_norm_g) * jax.nn.silu(z.reshape(bn_, ln_, DN_HEADS, DN_DV))
        y_lru = h_lru.astype(z.dtype) * jax.nn.gelu(gr)
        return jnp.concatenate([o.reshape(bn_, ln_, DN_V), y_lru], axis=-1) @ w_out

    y_lat = finish(dn_lat, lru_lat, zg_l)
    y_ctx = finish(dn_ctx, lru_ctx, zg_c) if ctx_out else None
    return y_ctx, y_lat


def odd_mixer(h_ctx, h_lat, w_in, gla_wg2, gla_bg, gla_norm_g, w_out, ctx_out):
    rows = h_lat.shape[1] // GRID_W
    h_lat = raster_transpose(h_lat, rows, GRID_W)

    def prepare(h):
        bn, ln, _ = h.shape
        q, k, v, gout, gdown = split_cols(h @ w_in, (GLA_QK, GLA_QK, GLA_V, GLA_V, 2 * GLA_RANK))
        heads = lambda t: t.reshape(bn, ln, GLA_HEADS, -1).transpose(0, 2, 1, 3)
        logit = jnp.einsum('bldr,drk->dblk', gdown.reshape(bn, ln, 2, GLA_RANK), gla_wg2) + gla_bg[:, None, None]
        glog = jax.nn.log_sigmoid(logit.astype(jnp.float32)) / GLA_TAU
        glog = glog.reshape(2, bn, ln, GLA_HEADS, GLA_DK).transpose(0, 1, 3, 2, 4)
        return (heads(q) * GLA_DK ** -0.5, heads(k), heads(v)), (glog,), gout

    qkv_c, g_c, gout_c = prepare(h_ctx)
    qkv_l, g_l, gout_l = prepare(h_lat)
    bn = h_lat.shape[0]
    o_ctx, o_lat = bidirectional(gla_chunked, qkv_c, g_c, qkv_l, g_l,
                                 jnp.zeros((bn, GLA_HEADS, GLA_DK, GLA_DV), h_lat.dtype), 2)

    def finish(o, gout):
        bn_, ln_ = gout.shape[:2]
        o = rms_norm(o.transpose(0, 2, 1, 3), gla_norm_g) * jax.nn.silu(gout.reshape(bn_, ln_, GLA_HEADS, GLA_DV))
        return o.reshape(bn_, ln_, GLA_V) @ w_out

    y_lat = raster_transpose(finish(o_lat, gout_l), GRID_W, rows)
    y_ctx = finish(o_ctx, gout_c) if ctx_out else None
    return y_ctx, y_lat


def swiglu(h, w_gate, w_up, w_down):
    return (jax.nn.silu(h @ w_gate) * (h @ w_up)) @ w_down


def moe_swiglu(x, router_w, router_b, w_gate, w_up, w_down):
    n_tok, d = x.shape
    logits = (x @ router_w + router_b).astype(jnp.float32)
    top_logit, top_idx = lax.top_k(logits, TOP_K)
    top_w = jax.nn.softmax(top_logit, axis=-1).astype(x.dtype)
    n_assign = n_tok * TOP_K
    e_flat = top_idx.reshape(-1)
    tok_flat = jnp.arange(n_assign, dtype=jnp.int32) // TOP_K
    w_flat = top_w.reshape(-1)
    order = jnp.argsort(e_flat)
    e_sorted = e_flat[order]
    counts = jnp.bincount(e_flat, length=N_EXPERTS)
    padded = (counts + MOE_BLOCK - 1) // MOE_BLOCK * MOE_BLOCK
    pad_end = jnp.cumsum(padded)
    pad_start = pad_end - padded
    raw_start = jnp.cumsum(counts) - counts
    dest = pad_start[e_sorted] + jnp.arange(n_assign, dtype=jnp.int32) - raw_start[e_sorted]
    n_blocks = -(-n_assign // MOE_BLOCK) + N_EXPERTS
    n_slots = n_blocks * MOE_BLOCK
    slot_tok = jnp.full((n_slots,), n_tok, jnp.int32).at[dest].set(tok_flat[order])
    slot_w = jnp.zeros((n_slots,), x.dtype).at[dest].set(w_flat[order])
    block_expert = jnp.minimum(
        jnp.searchsorted(pad_end, jnp.arange(n_blocks, dtype=jnp.int32) * MOE_BLOCK, side='right'), N_EXPERTS - 1)
    x_pad = jnp.concatenate([x, jnp.zeros((1, d), x.dtype)], axis=0)
    xb = x_pad[slot_tok].reshape(n_blocks, MOE_BLOCK, d)

    def expert_block(args):
        xb_i, e = args
        return swiglu(xb_i, w_gate[e], w_up[e], w_down[e])

    yb = lax.map(expert_block, (xb, block_expert)).reshape(n_slots, d)
    y = jnp.zeros((n_tok + 1, d), x.dtype).at[slot_tok].add(yb * slot_w[:, None])
    return y[:n_tok]


def setup_inputs(seed: int = 0) -> dict:
    key = jax.random.key(seed)
    ks = iter(jax.random.split(key, 48))
    D = D_MODEL
    f32 = jnp.float32
    nrm = lambda shape, scale: jax.random.normal(next(ks), shape, f32) * scale
    gain = lambda shape: 1.0 + nrm(shape, 0.02)
    u_lam = jax.random.uniform(next(ks), (N_EVEN, 2, LRU_WIDTH), f32, 0.9, 0.999)
    a_lam = u_lam ** (1.0 / LRU_C)
    lam = jnp.log(a_lam) - jnp.log1p(-a_lam)
    dt = jnp.exp(jax.random.uniform(next(ks), (N_EVEN, 2, DN_HEADS), f32, math.log(1e-3), math.log(1e-1)))
    dt_bias = dt + jnp.log(-jnp.expm1(-dt))
    a_log = jnp.log(jax.random.uniform(next(ks), (N_EVEN, 2, DN_HEADS), f32, 1.0, 16.0))
    return {
        'x': nrm((BATCH, SEQ, D), 1.0),
        'c': nrm((BATCH, D), 1.0),
        'ctx': nrm((BATCH, CTX_LEN, D), 1.0),
        'c_ctx': nrm((D,), 1.0),
        'mod_w': nrm((DEPTH, D, N_MOD * D), 0.5 * D ** -0.5),
        'mod_b': nrm((DEPTH, N_MOD * D), 0.01),
        'norm1_g': gain((DEPTH, D)),
        'norm2_g': gain((DEPTH, D)),
        'ev_w_in': nrm((N_EVEN, D, EV_IN), D ** -0.5),
        'ev_conv_qkv': nrm((N_EVEN, CONV_W, 2 * DN_QK + DN_V), CONV_W ** -0.5),
        'ev_dn_a_log': a_log,
        'ev_dn_dt_bias': dt_bias,
        'ev_dn_norm_g': gain((N_EVEN, DN_DV)),
        'ev_lru_conv_w': nrm((N_EVEN, CONV_W, LRU_WIDTH), CONV_W ** -0.5),
        'ev_lru_conv_b': nrm((N_EVEN, LRU_WIDTH), 0.01),
        'ev_lru_wa': nrm((N_EVEN, 2, LRU_BLOCKS, LRU_BW, LRU_BW), LRU_BW ** -0.5),
        'ev_lru_ba': nrm((N_EVEN, 2, LRU_WIDTH), 0.01),
        'ev_lru_wx': nrm((N_EVEN, 2, LRU_BLOCKS, LRU_BW, LRU_BW), LRU_BW ** -0.5),
        'ev_lru_bx': nrm((N_EVEN, 2, LRU_WIDTH), 0.01),
        'ev_lru_lambda': lam,
        'ev_w_out': nrm((N_EVEN, EV_MIX, D), EV_MIX ** -0.5),
        'ev_ffn_w_gate': nrm((N_EVEN, D, FFN_HIDDEN), D ** -0.5),
        'ev_ffn_w_up': nrm((N_EVEN, D, FFN_HIDDEN), D ** -0.5),
        'ev_ffn_w_down': nrm((N_EVEN, FFN_HIDDEN, D), FFN_HIDDEN ** -0.5),
        'od_w_in': nrm((N_ODD, D, OD_IN), D ** -0.5),
        'od_gla_wg2': nrm((N_ODD, 2, GLA_RANK, GLA_QK), GLA_RANK ** -0.5),
        'od_gla_bg': nrm((N_ODD, 2, GLA_QK), 0.01),
        'od_gla_norm_g': gain((N_ODD, GLA_DV)),
        'od_w_out': nrm((N_ODD, GLA_V, D), GLA_V ** -0.5),
        'od_router_w': nrm((N_ODD, D, N_EXPERTS), D ** -0.5),
        'od_router_b': nrm((N_ODD, N_EXPERTS), 0.01),
        'od_exp_w_gate': nrm((N_ODD, N_EXPERTS, D, FFN_HIDDEN), D ** -0.5),
        'od_exp_w_up': nrm((N_ODD, N_EXPERTS, D, FFN_HIDDEN), D ** -0.5),
        'od_exp_w_down': nrm((N_ODD, N_EXPERTS, FFN_HIDDEN, D), FFN_HIDDEN ** -0.5),
        'final_norm_g': gain((D,)),
    }


def reference(x, c, ctx, c_ctx, mod_w, mod_b, norm1_g, norm2_g, ev_w_in, ev_conv_qkv, ev_dn_a_log, ev_dn_dt_bias,
              ev_dn_norm_g, ev_lru_conv_w, ev_lru_conv_b, ev_lru_wa, ev_lru_ba, ev_lru_wx, ev_lru_bx, ev_lru_lambda,
              ev_w_out, ev_ffn_w_gate, ev_ffn_w_up, ev_ffn_w_down, od_w_in, od_gla_wg2, od_gla_bg, od_gla_norm_g,
              od_w_out, od_router_w, od_router_b, od_exp_w_gate, od_exp_w_up, od_exp_w_down, final_norm_g):
    b_, length, d = x.shape
    for layer in range(DEPTH):
        i = layer // 2
        last = layer == DEPTH - 1
        sh1, sc1, gt1, sh2, sc2, gt2 = adaln(c, mod_w[layer], mod_b[layer])
        csh1, csc1, cgt1, csh2, csc2, cgt2 = adaln(c_ctx[None], mod_w[layer], mod_b[layer])
        h_lat = modulate(rms_norm(x, norm1_g[layer]), sh1, sc1)
        h_ctx = modulate(rms_norm(ctx, norm1_g[layer]), csh1, csc1)
        if layer % 2 == 0:
            y_ctx, y_lat = even_mixer(h_ctx, h_lat, ev_w_in[i], ev_conv_qkv[i], ev_dn_a_log[i], ev_dn_dt_bias[i],
                                      ev_dn_norm_g[i], ev_lru_conv_w[i], ev_lru_conv_b[i], ev_lru_wa[i], ev_lru_ba[i],
                                      ev_lru_wx[i], ev_lru_bx[i], ev_lru_lambda[i], ev_w_out[i], not last)
            ffn = lambda t: swiglu(t, ev_ffn_w_gate[i], ev_ffn_w_up[i], ev_ffn_w_down[i])
        else:
            y_ctx, y_lat = odd_mixer(h_ctx, h_lat, od_w_in[i], od_gla_wg2[i], od_gla_bg[i], od_gla_norm_g[i],
                                     od_w_out[i], not last)
            ffn = lambda t: moe_swiglu(t.reshape(-1, d), od_router_w[i], od_router_b[i], od_exp_w_gate[i],
                                       od_exp_w_up[i], od_exp_w_down[i]).reshape(t.shape)
        x = x + gt1 * y_lat
        x = x + gt2 * ffn(modulate(rms_norm(x, norm2_g[layer]), sh2, sc2))
        if not last:
            ctx = ctx + cgt1 * y_ctx
            ctx = ctx + cgt2 * ffn(modulate(rms_norm(ctx, norm2_g[layer]), csh2, csc2))
    return rms_norm(x, final_norm_g)
```

```python
import numpy as np
import concourse.bass as bass
import concourse.mybir as mybir
from concourse.bass_utils import run_bass_kernel_spmd

F32 = mybir.dt.float32
BF16 = mybir.dt.bfloat16
AF = mybir.ActivationFunctionType
ALU = mybir.AluOpType

D = 2048
KC = 16
FFN_H = 5632
HB = 44
EPS = 1e-6
NCORES = 8
CTX = 256
GRID_W = 64


class Prog:
    CAP = 30000
    DCAP = 1800
    NDS = 8

    def __init__(self):
        self.nc = bass.Bass("TRN2", target_bir_lowering=False)
        nc = self.nc
        self.eng = {'pe': nc.tensor, 'dve': nc.vector, 'act': nc.scalar, 'pool': nc.gpsimd, 'sp': nc.sync}
        self.sems = {k: [] for k in self.eng}
        self.cnt = {k: 0 for k in self.eng}
        self.dsems = {}
        self.dcnt = {}
        self.dn = {k: 0 for k in self.eng}
        self.waited = {k: {} for k in self.eng}
        self.last_w = {}
        self.reads = {}
        self.nsem = 0
        self.ninstr = 0
        self.nalloc = 0
        self.used = set()
        self.needed = Prog.NEEDED

    def sb(self, shape, dtype=F32, name=None):
        self.nalloc += 1
        st = getattr(self, '_stack', None)
        if st is not None:
            return st.enter_context(self.nc.sbuf_tensor(name or f"sb{self.nalloc}", list(shape), dtype))
        return self.nc.alloc_sbuf_tensor(name or f"sb{self.nalloc}", list(shape), dtype)

    def scope(self):
        import contextlib
        prog = self

        @contextlib.contextmanager
        def cm():
            st = contextlib.ExitStack()
            prev = getattr(prog, '_stack', None)
            prog._stack = st
            try:
                yield
            finally:
                prog.barrier()
                prog._stack = prev
                st.close()
        return cm()

    def ps(self, shape, dtype=F32, name=None):
        self.nalloc += 1
        return self.nc.alloc_psum_tensor(name or f"ps{self.nalloc}", list(shape), dtype)

    def din(self, name, shape, dtype=F32):
        return self.nc.dram_tensor(name, list(shape), dtype, kind="ExternalInput").ap()

    def dout(self, name, shape, dtype=F32):
        return self.nc.dram_tensor(name, list(shape), dtype, kind="ExternalOutput").ap()

    def dscr(self, name, shape, dtype=F32):
        return self.nc.dram_tensor(name, list(shape), dtype, kind="Internal").ap()

    NEEDED = None

    def _newsem(self, nm):
        self.nsem += 1
        return self.nc.alloc_semaphore(name=f"{nm}_{self.nsem}")

    def _csem(self, e, rank):
        ep, v = divmod(rank, self.CAP)
        while len(self.sems[e]) <= ep:
            self.sems[e].append(self._newsem(e))
        return self.sems[e][ep], v + 1

    def _resolve(self, ticket):
        if ticket[0] == 'd':
            return ticket[1], ticket[2]
        _, e, idx = ticket
        self.used.add((e, idx))
        if self.needed is None:
            rank = idx
        else:
            rank = self.needed[e][idx]
        return self._csem(e, rank)

    def _wait_many(self, e, tickets):
        cbest = {}
        dbest = {}
        for t in tickets:
            if t is None:
                continue
            if t[0] == 'c':
                if e == 'pe' and t[1] == 'pe':
                    continue
                if cbest.get(t[1], -1) < t[2]:
                    cbest[t[1]] = t[2]
            else:
                k = id(t[1])
                if k not in dbest or dbest[k][1] < t[2]:
                    dbest[k] = (t[1], t[2])
        for e2, idx in cbest.items():
            if self.waited[e].get(e2, -1) >= idx:
                continue
            sem, val = self._resolve(('c', e2, idx))
            self.eng[e].wait_ge(sem, val)
            self.waited[e][e2] = idx
        for k, (sem, val) in dbest.items():
            if self.waited[e].get(k, 0) >= val:
                continue
            self.eng[e].wait_ge(sem, val)
            self.waited[e][k] = val

    def _wait(self, e, ticket):
        self._wait_many(e, [ticket])

    def _deps(self, e, reads, writes):
        ts = [self.last_w.get(r) for r in list(reads) + list(writes)]
        for w in writes:
            ts.extend(self.reads.get(w, []))
        self._wait_many(e, ts)

    def _record(self, ticket, reads, writes):
        for r in reads:
            self.reads.setdefault(r, []).append(ticket)
        for w in writes:
            self.last_w[w] = ticket
            self.reads[w] = []

    def op(self, e, reads, writes, fn):
        self._deps(e, reads, writes)
        idx = self.cnt[e]
        ins = fn()
        if self.needed is None:
            sem, val = self._csem(e, idx)
            ins.then_inc(sem, 1)
        elif idx in self.needed[e]:
            sem, val = self._csem(e, self.needed[e][idx])
            ins.then_inc(sem, 1)
        self.cnt[e] = idx + 1
        self.ninstr += 1
        t = ('c', e, idx)
        self._record(t, reads, writes)
        return t

    def dma(self, out, in_, reads=None, writes=None, q='sp', **kw):
        reads, writes = self._rw([in_], [out])
        self._deps(q, reads, writes)
        n = self.dn[q]
        self.dn[q] = n + 1
        key = (q, n % self.NDS)
        c = self.dcnt.get(key, 0)
        ep, v = divmod(c, self.DCAP)
        lst = self.dsems.setdefault(key, [])
        while len(lst) <= ep:
            lst.append(self._newsem('d' + q))
        sem = lst[ep]
        if v > 0:
            self._wait(q, ('d', sem, 16 * v))
        elif ep > 0:
            self._wait(q, ('d', lst[ep - 1], 16 * self.DCAP))
        self.eng[q].dma_start(out=out, in_=in_, **kw).then_inc(sem, 16)
        self.dcnt[key] = c + 1
        self.ninstr += 1
        t = ('d', sem, 16 * (v + 1))
        self._record(t, reads, writes)
        return t

    def finish(self, outs):
        for r in outs:
            self._wait('sp', self.last_w.get(r if isinstance(r, str) else self._nm(r)))

    def barrier(self):
        tickets = []
        for e2 in self.eng:
            if self.cnt[e2] > 0:
                tickets.append(('c', e2, self.cnt[e2] - 1))
        for key, c in self.dcnt.items():
            if c > 0:
                ep, v = divmod(c - 1, self.DCAP)
                tickets.append(('d', self.dsems[key][ep], 16 * (v + 1)))
        for e in self.eng:
            self._wait_many(e, [t for t in tickets if not (t[0] == 'c' and t[1] == e)])

    def bank(self, i):
        if not hasattr(self, '_banks'):
            self._banks = [self.ps([128, 512], name=f"bank{j}") for j in range(8)]
        return self._banks[i]

    @staticmethod
    def _nm(x):
        try:
            return x.tensor.name
        except AttributeError:
            return None

    def _rw(self, ins, outs):
        r = [n for n in (self._nm(a) for a in ins) if n is not None]
        w = [n for n in (self._nm(a) for a in outs) if n is not None]
        return r, w

    def mm(self, out, lhsT, rhs, reads=None, writes=None, start=True, stop=True):
        nc = self.nc
        r, w = self._rw([lhsT, rhs], [out])
        return self.op('pe', r, w, lambda: nc.tensor.matmul(out, lhsT, rhs, start=start, stop=stop))

    def tr(self, out, in_, ident, reads=None, writes=None):
        nc = self.nc
        r, w = self._rw([in_, ident], [out])
        return self.op('pe', r, w, lambda: nc.tensor.transpose(out, in_, ident))

    def act(self, out, in_, func, reads=None, writes=None, **kw):
        nc = self.nc
        r, w = self._rw([in_, kw.get('scale'), kw.get('bias')], [out, kw.get('accum_out')])
        return self.op('act', r, w, lambda: nc.scalar.activation(out=out, in_=in_, func=func, **kw))

    def ts(self, out, in0, s1, s2, op0, op1, reads=None, writes=None, e='dve'):
        eng = self.eng[e]
        r, w = self._rw([in0, s1, s2], [out])
        if op1 is None:
            return self.op(e, r, w, lambda: eng.tensor_scalar(out=out, in0=in0, scalar1=s1, scalar2=None, op0=op0))
        return self.op(e, r, w, lambda: eng.tensor_scalar(out=out, in0=in0, scalar1=s1, scalar2=s2, op0=op0, op1=op1))

    def tt(self, out, in0, in1, op, reads=None, writes=None, e='dve'):
        eng = self.eng[e]
        r, w = self._rw([in0, in1], [out])
        return self.op(e, r, w, lambda: eng.tensor_tensor(out=out, in0=in0, in1=in1, op=op))

    def stt(self, out, in0, scalar, in1, op0, op1, reads=None, writes=None):
        nc = self.nc
        r, w = self._rw([in0, scalar, in1], [out])
        return self.op('dve', r, w, lambda: nc.vector.scalar_tensor_tensor(out=out, in0=in0, scalar=scalar, in1=in1, op0=op0, op1=op1))

    def cp(self, out, in_, reads=None, writes=None, e='dve'):
        eng = self.eng[e]
        if e == 'act':
            return self.act(out, in_, AF.Copy)
        r, w = self._rw([in_], [out])
        return self.op(e, r, w, lambda: eng.tensor_copy(out=out, in_=in_))

    def memset(self, ap, val, writes=None, e='pool'):
        eng = self.eng[e]
        r, w = self._rw([], [ap])
        return self.op(e, r, w, lambda: eng.memset(ap, val))

    def recip(self, out, in_, reads=None, writes=None):
        nc = self.nc
        r, w = self._rw([in_], [out])
        return self.op('dve', r, w, lambda: nc.vector.reciprocal(out=out, in_=in_))

    def scan(self, out, d0, d1, init):
        nc = self.nc
        r, w = self._rw([d0, d1, init], [out])
        return self.op('dve', r, w, lambda: nc.vector.tensor_tensor_scan(out=out, data0=d0, data1=d1, initial=init, op0=ALU.mult, op1=ALU.add))

    def asel(self, t, pattern, cmp, fill, cm):
        nc = self.nc
        r, w = self._rw([t], [t])
        return self.op('pool', r, w, lambda: nc.gpsimd.affine_select(out=t, in_=t, pattern=pattern, compare_op=cmp, fill=fill, base=0, channel_multiplier=cm))

    def make_consts(self):
        nc = self.nc
        c = {}
        ident = self.sb([128, 128]); c['ident'] = ident
        self.memset(ident[:], 1.0, ['ident'])
        self.asel(ident[:], [[1, 128]], ALU.is_equal, 0.0, -1)
        ones = self.sb([128, 128]); c['ones'] = ones
        self.memset(ones[:], 1.0, ['ones'])
        def tri(name, fillv, keepv, cmp, cm, st):
            t = self.sb([128, 128]); c[name] = t
            self.memset(t[:], keepv, [name])
            self.asel(t[:], [[st, 128]], cmp, fillv, cm)
        tri('inc_f', 0.0, 1.0, ALU.is_ge, -1, 1)
        tri('inc_r', 0.0, 1.0, ALU.is_ge, 1, -1)
        NEG = -30000.0
        tri('neg_f', NEG, 0.0, ALU.is_ge, -1, 1)
        tri('neg_r', NEG, 0.0, ALU.is_ge, 1, -1)
        tri('negs_f', NEG, 0.0, ALU.is_gt, -1, 1)
        tri('negs_r', NEG, 0.0, ALU.is_gt, 1, -1)
        self.c = c
        self.barrier()
        return c


def two_pass(build, *args, **kw):
    Prog.NEEDED = None
    P1 = build(*args, **kw)
    needed = {e: {} for e in P1.eng}
    for (e, idx) in sorted(P1.used):
        needed[e][idx] = len(needed[e])
    Prog.NEEDED = needed
    try:
        P2 = build(*args, **kw)
    finally:
        Prog.NEEDED = None
    return P2


def _run(P, in_maps):
    res = run_bass_kernel_spmd(P.nc, in_maps, core_ids=list(range(len(in_maps))))
    return res.results


def fm16(v):
    return np.ascontiguousarray(np.asarray(v, np.float32).reshape(KC, 128).T)


def build_mod(depth, ncol):
    P = Prog(); nc = P.nc
    condT = P.din("condT", [128, KC * 2])
    w = P.din("w", [depth, D, ncol])
    b = P.din("b", [depth, 2, ncol])
    out = P.dout("out", [depth, 2, ncol])
    sc = P.sb([128, KC * 2])
    P.dma(sc[:], condT[:, :], [], ['sc'])
    P.act(sc[:], sc[:], AF.Silu, ['sc'], ['sc'])
    wt = P.sb([128, KC, ncol])
    bt = P.sb([2, ncol])
    ot = P.sb([2, ncol])
    nps = (ncol + 511) // 512
    pss = [P.ps([2, 512]) for _ in range(nps)]
    for l in range(depth):
        P.dma(wt[:], w[l].rearrange("(k p) n -> p k n", p=128), ['wt_free'], ['wt'])
        P.dma(bt[:], b[l], [], ['bt'])
        for g in range(nps):
            n0 = g * 512; n1 = min(ncol, n0 + 512)
            for kc in range(KC):
                P.mm(pss[g][:, 0:n1 - n0], sc[:, kc * 2:kc * 2 + 2], wt[:, kc, n0:n1], ['sc', 'wt'], [f'mps{g}'],
                     start=(kc == 0), stop=(kc == KC - 1))
            P.tt(ot[:, n0:n1], pss[g][:, 0:n1 - n0], bt[:, n0:n1], ALU.add, [f'mps{g}', 'bt'], ['ot'])
        P.dma(out[l], ot[:], ['ot'], ['out'])
    P.finish([out])
    return P


def run_mod(c, c_ctx, mod_w, mod_b):
    depth = mod_w.shape[0]
    ncol = mod_w.shape[2] // NCORES
    cond = np.stack([np.asarray(c).reshape(-1), np.asarray(c_ctx).reshape(-1)], 0)
    condT = np.ascontiguousarray(cond.reshape(2, KC, 128).transpose(2, 1, 0).reshape(128, KC * 2))
    P = two_pass(build_mod, depth, ncol)
    maps = []
    for j in range(NCORES):
        maps.append({"condT": condT,
                     "w": np.ascontiguousarray(mod_w[:, :, j * ncol:(j + 1) * ncol]),
                     "b": np.ascontiguousarray(np.broadcast_to(mod_b[:, None, j * ncol:(j + 1) * ncol], (depth, 2, ncol)))})
    res = _run(P, maps)
    m = np.concatenate([r["out"] for r in res], axis=2)
    return m.reshape(depth, 2, 6, D)


class NormProj:
    def __init__(self, P, ncond, keep32=False):
        self.P = P
        self.keep32 = keep32
        self.hT32 = P.sb([128, KC, 128]) if keep32 else None
        nc = P.nc
        self.xt = P.sb([128, D]); self.xn = P.sb([128, D]); self.junk = P.sb([128, D], BF16)
        self.ss = P.sb([128, 1]); self.rs = P.sb([128, 1])
        self.hT = P.sb([128, KC, 128], BF16)
        self.pT = [P.bank(i) for i in range(4)]
        self.g_in = P.din("np_g", [128, KC])
        self.sc_in = P.din("np_sc", [ncond, 128, KC])
        self.sh_in = P.din("np_sh", [ncond, 128, KC])
        self.ncond = ncond
        gt = P.sb([128, KC])
        self.msc = [P.sb([128, KC]) for _ in range(ncond)]
        self.msh = [P.sb([128, KC]) for _ in range(ncond)]
        P.dma(gt[:], self.g_in[:, :], [], ['np_gt'])
        for c in range(ncond):
            P.dma(self.msc[c][:], self.sc_in[c], [], [f'np_msc{c}'])
            P.dma(self.msh[c][:], self.sh_in[c], [], [f'np_msh{c}'])
            P.stt(self.msc[c][:], self.msc[c][:], 1.0, gt[:], ALU.add, ALU.mult, ['np_gt', f'np_msc{c}'], [f'np_msc{c}'])

    def tile(self, x_rows, cond):
        P = self.P; nc = P.nc
        P.dma(self.xt[:], x_rows, [], ['np_xt'])
        ss, rs = self.ss, self.rs
        P.act(self.junk[:], self.xt[:], AF.Square, ['np_xt'], ['np_junk', 'np_ss'], accum_out=ss[:])
        P.ts(rs[:], ss[:], 1.0 / D, EPS, ALU.mult, ALU.add, ['np_ss'], ['np_rs'])
        P.act(rs[:], rs[:], AF.Sqrt, ['np_rs'], ['np_rs'])
        P.recip(rs[:], rs[:], ['np_rs'], ['np_rs'])
        P.act(self.xn[:], self.xt[:], AF.Identity, ['np_xt', 'np_rs'], ['np_xn'], scale=rs[:, 0:1])
        ident = P.c['ident']
        for kc in range(KC):
            b, o = divmod(kc, 4)
            P.tr(self.pT[b][:, o * 128:(o + 1) * 128], self.xn[:, kc * 128:(kc + 1) * 128], ident[:], ['np_xn', 'ident'], [f'np_pT{b}'])
        msc, msh = self.msc[cond], self.msh[cond]
        dst = self.hT32 if self.keep32 else self.hT
        for kc in range(KC):
            b, o = divmod(kc, 4)
            rd = [f'np_pT{b}', f'np_msc{cond}', f'np_msh{cond}']
            if kc % 2 == 0:
                P.ts(dst[:, kc, :], self.pT[b][:, o * 128:(o + 1) * 128], msc[:, kc:kc + 1], msh[:, kc:kc + 1],
                     ALU.mult, ALU.add, rd, [f'hT{kc}'])
            else:
                P.act(dst[:, kc, :], self.pT[b][:, o * 128:(o + 1) * 128], AF.Identity, rd, [f'hT{kc}'],
                      scale=msc[:, kc:kc + 1], bias=msh[:, kc:kc + 1])
        if self.keep32:
            for b in range(4):
                P.cp(self.hT[:, b * 4:(b + 1) * 4, :], self.hT32[:, b * 4:(b + 1) * 4, :], e=('pool' if b % 2 else 'act'))


def load_w_bf16(P, w_dram, ncols, name):
    wt = P.sb([128, KC, ncols], BF16)
    stg = P.sb([128, 4, ncols])
    for q in range(KC // 4):
        P.dma(stg[:], w_dram[q * 512:(q + 1) * 512, :].rearrange("(k p) n -> p k n", p=128), [], [name + '_stg'])
        P.cp(wt[:, q * 4:(q + 1) * 4, :], stg[:], [name + '_stg'], [name], e=('dve' if q % 2 == 0 else 'pool'))
    return wt


def build_mixer_even(NT, NCT, phases='ABCD'):
    P = Prog(); nc = P.nc
    c = P.make_consts()
    ident, ones = c['ident'], c['ones']
    NTOK = NT * 128
    x_all = P.din("x_all", [NTOK, D])
    w_fm_d = P.din("w_fm", [D, 640])
    w_tm_d = P.din("w_tm", [D, 132])
    convw_d = P.din("convw", [128, 16])
    convb_d = P.din("convb", [128, 1])
    lruw_d = P.din("lruw", [4, 128, 128])
    lrub_d = P.din("lrub", [128, 4])
    lam_d = P.din("lam", [128, 2])
    dnp_d = P.din("dnp", [128, 4])
    dng_d = P.din("dng", [128, 128])
    out_dn = P.dout("out_dn", [NTOK, 128])
    out_lru = P.dout("out_lru", [128, NTOK])

    COFF = 4; LOFF = 4 + NCT * 128 + 4
    NPAD = LOFF + (NT - NCT) * 128 + 4
    S_fm = P.dscr("S_fm", [5, 128, NPAD])
    S_z = P.dscr("S_z", [NTOK, 128])
    S_ab = P.dscr("S_ab", [NTOK, 4])
    S_qT = P.dscr("S_qT", [NT, 128, 128]); S_kT = P.dscr("S_kT", [NT, 128, 128])
    S_k = P.dscr("S_k", [NT, 128, 128]); S_v = P.dscr("S_v", [NT, 128, 128])
    S_g = P.dscr("S_g", [NTOK, 4])
    S_a = P.dscr("S_a", [2, 128, NTOK]); S_b = P.dscr("S_b", [2, 128, NTOK]); S_gg = P.dscr("S_gg", [128, NTOK])
    S_o = P.dscr("S_o", [NTOK, 128])

    def col0(t):
        return (COFF + t * 128) if t < NCT else (LOFF + (t - NCT) * 128)

    zt = P.sb([128, 5, 4])
    P.memset(zt[:], 0.0, ['zt'])
    for off in (0, COFF + NCT * 128, LOFF + (NT - NCT) * 128):
        P.dma(S_fm[:, :, off:off + 4].rearrange("g p n -> p g n"), zt[:], ['zt'], ['S_fm'])

    npj = NormProj(P, 2)
    w_fm = load_w_bf16(P, w_fm_d, 640, 'w_fm')
    w_tm = load_w_bf16(P, w_tm_d, 132, 'w_tm')
    pA = [P.bank(4), P.bank(5)]
    stgA = P.sb([128, 5, 128]); stgZ = P.sb([128, 128]); ab_all = P.sb([128, NT, 4]); g_all = P.sb([128, NT, 4])
    for t in (range(NT) if 'A' in phases else []):
        npj.tile(x_all[t * 128:(t + 1) * 128, :], 1 if t < NCT else 0)
        import os
        DBG = int(os.environ.get('DBG', '9'))
        if DBG < 2: continue
        for g in range(5):
            b, o = divmod(g, 4)
            for kc in range(KC):
                P.mm(pA[b][:, o * 128:(o + 1) * 128], w_fm[:, kc, g * 128:(g + 1) * 128], npj.hT[:, kc, :], ['w_fm', f'hT{kc}'], [f'pA{b}'],
                     start=(kc == 0), stop=(kc == KC - 1))
        if DBG < 3: continue
        for kc in range(KC):
            P.mm(pA[1][:, 128:260], npj.hT[:, kc, :], w_tm[:, kc, :], ['w_tm', f'hT{kc}'], ['pA1'], start=(kc == 0), stop=(kc == KC - 1))
        if DBG < 4: continue
        P.cp(stgA[:, 0:4, :], pA[0][:, :].rearrange("p (g n) -> p g n", g=4), ['pA0'], ['stgA'])
        P.cp(stgA[:, 4, :], pA[1][:, 0:128], ['pA1'], ['stgA'], e='act')
        P.act(stgZ[:], pA[1][:, 128:256], AF.Silu, ['pA1'], ['stgZ'])
        P.cp(ab_all[:, t, :], pA[1][:, 256:260])
        if DBG < 5: continue
        c0 = col0(t)
        M5 = os.environ.get('M5', 'abc')
        if 'a' in M5: P.dma(S_fm[:, :, c0:c0 + 128].rearrange("g p n -> p g n"), stgA[:], ['stgA'], ['S_fm'])
        if 'b' in M5: P.dma(S_z[t * 128:(t + 1) * 128, :], stgZ[:], ['stgZ'], ['S_z'])


    convw = P.sb([128, 16]); convb = P.sb([128, 1]); lruw = P.sb([128, 4, 128]); lrub = P.sb([128, 4])
    lam = P.sb([128, 2]); dnp = P.sb([128, 4]); dng = P.sb([128, 128])
    P.dma(convw[:], convw_d[:, :], [], ['convw']); P.dma(convb[:], convb_d[:, :], [], ['convb'])
    P.dma(lruw[:], lruw_d.rearrange("g p n -> p g n"), [], ['lruw']); P.dma(lrub[:], lrub_d[:, :], [], ['lrub'])
    P.dma(lam[:], lam_d[:, :], [], ['lam']); P.dma(dnp[:], dnp_d[:, :], [], ['dnp']); P.dma(dng[:], dng_d[:, :], [], ['dng'])
    nsp8 = P.sb([128, 2]); nea = P.sb([128, 2])
    P.act(nsp8[:], lam[:], AF.Exp, ['lam'], ['nsp8'], scale=-1.0)
    P.act(nsp8[:], nsp8[:], AF.Ln, ['nsp8'], ['nsp8'], bias=1.0)
    P.ts(nsp8[:], nsp8[:], -8.0, None, ALU.mult, None, ['nsp8'], ['nsp8'])
    P.act(nea[:], dnp[:, 0:2], AF.Exp, ['dnp'], ['nea'])
    P.ts(nea[:], nea[:], -1.0, None, ALU.mult, None, ['nea'], ['nea'])

    pre = P.sb([128, 4, 131]); gr = P.sb([128, 128]); cv = P.sb([128, 4, 128]); sq = P.sb([128, 256]); rn = P.sb([128, 256])
    qk = P.sb([128, 2, 128]); ktm = P.sb([128, 128]); vtm = P.sb([128, 128])
    pB = P.bank(6); pB2 = P.bank(5)
    gate = P.sb([128, 4, 128]); av = P.sb([128, 2, 128]); bv = P.sb([128, 2, 128]); tmp = P.sb([128, 2, 128]); gg = P.sb([128, 128])
    for t in (range(NT) if 'B' in phases else []):
        c0 = col0(t)
        P.dma(pre[:], S_fm[0:4, :, c0 - 2:c0 + 129].rearrange("g p n -> p g n"), ['S_fm'], ['pre'])
        P.dma(gr[:], S_fm[4, :, c0:c0 + 128], ['S_fm'], ['gr'])
        for g in range(4):
            P.ts(cv[:, g, :], pre[:, g, 0:128], convw[:, g * 4:g * 4 + 1], None, ALU.mult, None, ['pre', 'convw'], ['cv'])
            for j in range(1, 4):
                P.stt(cv[:, g, :], pre[:, g, j:j + 128], convw[:, g * 4 + j:g * 4 + j + 1], cv[:, g, :], ALU.mult, ALU.add,
                      ['pre', 'convw', 'cv'], ['cv'])
        if float(os.environ.get('DBGB', '9')) < 1: continue
        P.act(cv[:, 0:3, :], cv[:, 0:3, :], AF.Silu, ['cv'], ['cv'])
        if float(os.environ.get('DBGB', '9')) < 1.2: continue
        P.act(sq[:], cv[:, 0:2, :].rearrange("p g n -> p (g n)"), AF.Square, ['cv'], ['sq'])
        if float(os.environ.get('DBGB', '9')) < 1.4: continue
        P.mm(pB[:, 0:256], ones[:], sq[:], ['ones', 'sq'], ['pB'])
        if float(os.environ.get('DBGB', '9')) < 1.5: continue
        P.ts(rn[:], pB[:, 0:256], EPS, None, ALU.add, None, ['pB'], ['rn'])
        if float(os.environ.get('DBGB', '9')) < 1.6: continue
        P.act(rn[:], rn[:], AF.Sqrt, ['rn'], ['rn'])
        P.recip(rn[:], rn[:], ['rn'], ['rn'])
        if float(os.environ.get('DBGB', '9')) < 1.8: continue
        P.stt(qk[:, 0, :], rn[:, 0:128], 128.0 ** -0.5, cv[:, 0, :], ALU.mult, ALU.mult, ['rn', 'cv'], ['qk'])
        P.tt(qk[:, 1, :], rn[:, 128:256], cv[:, 1, :], ALU.mult, ['rn', 'cv'], ['qk'])
        if float(os.environ.get('DBGB', '9')) < 2: continue
        P.tr(pB2[:, 0:128], qk[:, 1, :], ident[:], ['qk', 'ident'], ['pB2'])
        if float(os.environ.get('DBGB', '9')) < 2.2: continue
        P.tr(pB2[:, 128:256], cv[:, 2, :], ident[:], ['cv', 'ident'], ['pB2'])
        if float(os.environ.get('DBGB', '9')) < 2.4: continue
        if os.environ.get('KE', 'act') == 'ts':
            P.ts(ktm[:], pB2[:, 0:128], 1.0, None, ALU.mult, None)
        else:
            P.cp(ktm[:], pB2[:, 0:128], e=os.environ.get('KE', 'act'))
        if float(os.environ.get('DBGB', '9')) < 2.6: continue
        P.cp(vtm[:], pB2[:, 128:256], ['pB2'], ['vtm'], e='act')
        if float(os.environ.get('DBGB', '9')) < 3: continue
        P.dma(S_qT[t], qk[:, 0, :], ['qk'], ['S_qT']); P.dma(S_kT[t], qk[:, 1, :], ['qk'], ['S_kT'])
        P.dma(S_k[t], ktm[:], ['ktm'], ['S_k']); P.dma(S_v[t], vtm[:], ['vtm'], ['S_v'])
        if float(os.environ.get('DBGB', '9')) < 4: continue
        P.ts(cv[:, 3, :], cv[:, 3, :], convb[:, 0:1], None, ALU.add, None, ['cv', 'convb'], ['cv'])
        for g in range(4):
            P.mm(pB[:, 256:384] if g % 2 == 0 else pB[:, 384:512], lruw[:, g, :], cv[:, 3, :], ['lruw', 'cv'], [f'pBg{g % 2}'])
            P.act(gate[:, g, :], pB[:, 256:384] if g % 2 == 0 else pB[:, 384:512], AF.Sigmoid, [f'pBg{g % 2}', 'lrub'], ['gate'],
                  bias=lrub[:, g:g + 1])
        for d in range(2):
            P.act(av[:, d, :], gate[:, d, :], AF.Exp, ['gate', 'nsp8'], ['av'], scale=nsp8[:, d:d + 1])
        P.tt(tmp[:], av[:], av[:], ALU.mult, ['av'], ['tmp'])
        P.ts(tmp[:], tmp[:], -1.0, 1.0, ALU.mult, ALU.add, ['tmp'], ['tmp'])
        P.act(tmp[:], tmp[:], AF.Sqrt, ['tmp'], ['tmp'])
        P.tt(bv[:], tmp[:], gate[:, 2:4, :], ALU.mult, ['tmp', 'gate'], ['bv'])
        for d in range(2):
            P.tt(bv[:, d, :], bv[:, d, :], cv[:, 3, :], ALU.mult, ['bv', 'cv'], ['bv'])
        P.act(gg[:], gr[:], AF.Square, ['gr'], ['gg'])
        P.ts(gg[:], gg[:], 0.044715, 1.0, ALU.mult, ALU.add, ['gg'], ['gg'])
        P.tt(gg[:], gg[:], gr[:], ALU.mult, ['gg', 'gr'], ['gg'])
        P.act(gg[:], gg[:], AF.Sigmoid, ['gg'], ['gg'], scale=1.5957691216057308)
        P.tt(gg[:], gg[:], gr[:], ALU.mult, ['gg', 'gr'], ['gg'])
        if float(os.environ.get('DBGB', '9')) < 5: continue
        P.dma(S_a[:, :, t * 128:(t + 1) * 128].rearrange("d p n -> p d n"), av[:], ['av'], ['S_a'])
        P.dma(S_b[:, :, t * 128:(t + 1) * 128].rearrange("d p n -> p d n"), bv[:], ['bv'], ['S_b'])
        P.dma(S_gg[:, t * 128:(t + 1) * 128], gg[:], ['gg'], ['S_gg'])
        if float(os.environ.get('DBGB', '9')) < 6: continue
        ab = ab_all[:, t, :]; gl4 = g_all[:, t, :]
        P.tt(gl4[:, 0:2], ab[:, 0:2], dnp[:, 2:4], ALU.add, ['ab', 'dnp'], ['gl4'])
        P.ts(gl4[:, 2:4], ab[:, 2:4], -1.0, None, ALU.mult, None, ['ab'], ['gl4'])
        P.act(gl4, gl4, AF.Exp)
        P.act(gl4, gl4, AF.Ln, bias=1.0)
        P.tt(gl4[:, 0:2], gl4[:, 0:2], nea[:], ALU.mult, ['gl4', 'nea'], ['gl4'])
        P.ts(gl4[:, 2:4], gl4[:, 2:4], -1.0, None, ALU.mult, None, ['gl4'], ['gl4'])

    qT = P.sb([128, 128]); kT = P.sb([128, 128]); kt = P.sb([128, 128]); vt = P.sb([128, 128]); g4 = P.sb([128, 4]); zs = P.sb([128, 128])
    S = P.sb([128, 128])
    rows = P.sb([1, 3, 128])
    cols = P.sb([128, 6])
    glb = P.sb([128, 1])
    EAT = P.sb([128, 128]); EA = P.sb([128, 128]); EQT = P.sb([128, 128])
    Nb = [P.sb([128, 128]) for _ in range(2)]; NTb = [P.sb([128, 128]) for _ in range(2)]
    QKm = P.sb([128, 128]); Y = P.sb([128, 256]); WT = P.sb([128, 128]); vnew = P.sb([128, 128]); kdec = P.sb([128, 128])
    otmp = P.sb([128, 128]); o = P.sb([128, 128]); of = P.sb([128, 128]); oss = P.sb([128, 1]); ojunk = P.sb([128, 128])
    P.barrier()
    p_row = P.bank(0)[0:1, :]; p_col = P.bank(1); p_E = P.bank(2); p_KK = P.bank(3)
    p_Y = P.bank(4); p_N = P.bank(5)
    one_row = ones[0:1, :]
    for d in (range(2) if 'C' in phases else []):
        inc = c['inc_f'] if d == 0 else c['inc_r']
        neg = c['neg_f'] if d == 0 else c['neg_r']
        negs = c['negs_f'] if d == 0 else c['negs_r']
        negsT = c['negs_r'] if d == 0 else c['negs_f']
        incn = 'inc_f' if d == 0 else 'inc_r'
        order = list(range(NT)) if d == 0 else (list(range(NCT - 1, -1, -1)) + list(range(NT - 1, NCT - 1, -1)))
        P.memset(S[:], 0.0, ['S'])
        for t in order:
            P.dma(qT[:], S_qT[t], ['S_qT'], ['qT']); P.dma(kT[:], S_kT[t], ['S_kT'], ['kT'])
            P.dma(kt[:], S_k[t], ['S_k'], ['kt']); P.dma(vt[:], S_v[t], ['S_v'], ['vt'])
            P.cp(g4[:], g_all[:, t, :], e='pool')
            gcol = g4[:, d:d + 1]; lbcol = g4[:, 2 + d:3 + d]
            P.mm(p_row[:, 0:128], gcol, inc[:], ['g4', incn], ['p_row'])
            P.mm(p_row[:, 128:256], gcol, inc[:], ['g4', incn], ['p_row'], start=True, stop=False)
            P.mm(p_row[:, 128:256], lbcol, ident[:], ['g4', 'ident'], ['p_row'], start=False, stop=True)
            P.cp(rows[:, 0, :], p_row[:, 0:128], ['p_row'], ['rows'])
            P.ts(rows[:, 1, :], p_row[:, 0:128], -1.0, None, ALU.mult, None, ['p_row'], ['rows'])
            P.cp(rows[:, 2, :], p_row[:, 128:256], ['p_row'], ['rows'])
            P.mm(p_col[:, 0:1], inc[:], gcol, ['g4', incn], ['p_col'])
            P.mm(p_col[:, 1:2], ones[:], gcol, ['g4', 'ones'], ['p_col'])
            P.cp(cols[:, 5:6], p_col[:, 0:1], ['p_col'], ['cols'])
            P.cp(glb[:], p_col[:, 1:2], ['p_col'], ['glb'])
            P.act(cols[:, 0:1], lbcol, AF.Exp, ['g4'], ['cols'])
            P.act(cols[:, 1:2], cols[:, 5:6], AF.Exp, ['cols', 'g4'], ['cols'], bias=lbcol)
            P.act(cols[:, 2:3], cols[:, 5:6], AF.Exp, ['cols'], ['cols'])
            P.act(cols[:, 3:4], cols[:, 5:6], AF.Exp, ['cols', 'glb'], ['cols'], scale=-1.0, bias=glb[:, 0:1])
            P.act(cols[:, 4:5], glb[:], AF.Exp, ['glb'], ['cols'])
            P.mm(p_E[:, 0:128], one_row, rows[:, 2, :], ['ones', 'rows'], ['p_E0'], start=True, stop=False)
            P.mm(p_E[:, 0:128], rows[:, 1, :], one_row, ['ones', 'rows'], ['p_E0'], start=False, stop=False)
            P.mm(p_E[:, 0:128], ident[:], negs[:], ['ident'], ['p_E0'], start=False, stop=True)
            P.act(EAT[:], p_E[:, 0:128], AF.Exp, ['p_E0'], ['EAT'])
            P.mm(p_E[:, 128:256], rows[:, 2, :], one_row, ['ones', 'rows'], ['p_E1'], start=True, stop=False)
            P.mm(p_E[:, 128:256], one_row, rows[:, 1, :], ['ones', 'rows'], ['p_E1'], start=False, stop=False)
            P.mm(p_E[:, 128:256], ident[:], negsT[:], ['ident'], ['p_E1'], start=False, stop=True)
            P.act(EA[:], p_E[:, 128:256], AF.Exp, ['p_E1'], ['EA'])
            P.mm(p_E[:, 256:384], one_row, rows[:, 0, :], ['ones', 'rows'], ['p_E2'], start=True, stop=False)
            P.mm(p_E[:, 256:384], rows[:, 1, :], one_row, ['ones', 'rows'], ['p_E2'], start=False, stop=False)
            P.mm(p_E[:, 256:384], ident[:], neg[:], ['ident'], ['p_E2'], start=False, stop=True)
            P.act(EQT[:], p_E[:, 256:384], AF.Exp, ['p_E2'], ['EQT'])
            P.mm(p_KK[:, 0:128], kT[:], kT[:], ['kT'], ['p_KK0'])
            P.mm(p_KK[:, 128:256], kT[:], qT[:], ['kT', 'qT'], ['p_KK1'])
            P.stt(NTb[0][:], p_KK[:, 0:128], -1.0, EAT[:], ALU.mult, ALU.mult, ['p_KK0', 'EAT'], ['NT0'])
            P.stt(Nb[0][:], p_KK[:, 0:128], -1.0, EA[:], ALU.mult, ALU.mult, ['p_KK0', 'EA'], ['N0'])
            P.tt(QKm[:], p_KK[:, 128:256], EQT[:], ALU.mult, ['p_KK1', 'EQT'], ['QKm'])
            P.ts(Y[:, 0:128], vt[:], cols[:, 0:1], None, ALU.mult, None, ['vt', 'cols'], ['Y'])
            P.ts(Y[:, 128:256], kt[:], cols[:, 1:2], None, ALU.mult, None, ['kt', 'cols'], ['Y'])
            for l in range(7):
                a, b = l % 2, (l + 1) % 2
                P.mm(p_Y[:, 0:256], NTb[a][:], Y[:], [f'NT{a}', 'Y'], ['p_Y'])
                if l < 6:
                    P.mm(p_N[:, 0:128], NTb[a][:], Nb[a][:], [f'NT{a}', f'N{a}'], ['p_N0'])
                    P.mm(p_N[:, 128:256], Nb[a][:], NTb[a][:], [f'NT{a}', f'N{a}'], ['p_N1'])
                P.tt(Y[:], p_Y[:, 0:256], Y[:], ALU.add, ['p_Y', 'Y'], ['Y'])
                if l < 6:
                    P.cp(Nb[b][:], p_N[:, 0:128], ['p_N0'], [f'N{b}'], e='act')
                    P.cp(NTb[b][:], p_N[:, 128:256], ['p_N1'], [f'NT{b}'], e='act')
            P.tr(p_N[:, 256:384], Y[:, 128:256], ident[:], ['Y', 'ident'], ['p_N2'])
            P.cp(WT[:], p_N[:, 256:384], ['p_N2'], ['WT'], e='act')
            P.mm(p_Y[:, 256:384], WT[:], S[:], ['WT', 'S'], ['p_Y1'])
            P.tt(vnew[:], Y[:, 0:128], p_Y[:, 256:384], ALU.subtract, ['Y', 'p_Y1'], ['vnew'])
            P.mm(p_KK[:, 256:384], qT[:], S[:], ['qT', 'S'], ['p_KK2'])
            P.mm(p_KK[:, 384:512], QKm[:], vnew[:], ['QKm', 'vnew'], ['p_KK3'])
            P.cp(otmp[:], p_KK[:, 384:512], ['p_KK3'], ['otmp'], e='act')
            P.stt(o[:], p_KK[:, 256:384], cols[:, 2:3], otmp[:], ALU.mult, ALU.add, ['p_KK2', 'cols', 'otmp'], ['o'])
            P.ts(kdec[:], kt[:], cols[:, 3:4], None, ALU.mult, None, ['kt', 'cols'], ['kdec'])
            P.mm(p_Y[:, 384:512], kdec[:], vnew[:], ['kdec', 'vnew'], ['p_Y2'])
            P.stt(S[:], S[:], cols[:, 4:5], p_Y[:, 384:512], ALU.mult, ALU.add, ['S', 'cols', 'p_Y2'], ['S'])
            if d == 0:
                P.dma(S_o[t * 128:(t + 1) * 128, :], o[:], ['o'], ['S_o'])
            else:
                P.dma(of[:], S_o[t * 128:(t + 1) * 128, :], ['S_o'], ['of'])
                P.dma(zs[:], S_z[t * 128:(t + 1) * 128, :], ['S_z'], ['zs'])
                P.tt(o[:], o[:], of[:], ALU.add, ['o', 'of'], ['o'])
                P.act(ojunk[:], o[:], AF.Square, ['o'], ['ojunk', 'oss'], accum_out=oss[:])
                P.ts(oss[:], oss[:], 1.0 / 128, EPS, ALU.mult, ALU.add, ['oss'], ['oss'])
                P.act(oss[:], oss[:], AF.Sqrt, ['oss'], ['oss'])
                P.recip(oss[:], oss[:], ['oss'], ['oss'])
                P.stt(o[:], o[:], oss[:, 0:1], dng[:], ALU.mult, ALU.mult, ['o', 'oss', 'dng'], ['o'])
                P.tt(o[:], o[:], zs[:], ALU.mult, ['o', 'zs'], ['o'])
                P.dma(out_dn[t * 128:(t + 1) * 128, :], o[:], ['o'], ['out_dn'])

    BL = min(2048, (NT - NCT) * 128)
    hsum = P.sb([128, NTOK]); ab_a = P.sb([128, BL]); ab_b = P.sb([128, BL]); hb = P.sb([128, BL]); st = P.sb([128, 1])
    segs = [(0, NCT * 128)] + [(s, min(s + BL, NTOK)) for s in range(NCT * 128, NTOK, BL)]
    for d in (range(2) if 'D' in phases else []):
        sl = segs if d == 0 else ([segs[0]] + segs[1:][::-1])
        P.memset(st[:], 0.0, ['st'])
        for (s0, s1) in sl:
            n = s1 - s0
            P.dma(ab_a[:, 0:n], S_a[d, :, s0:s1], ['S_a'], ['ab_a'])
            P.dma(ab_b[:, 0:n], S_b[d, :, s0:s1], ['S_b'], ['ab_b'])
            if d == 0:
                P.scan(hsum[:, s0:s1], ab_a[:, 0:n], ab_b[:, 0:n], st[:, 0:1])
                P.cp(st[:], hsum[:, s1 - 1:s1], ['hsum'], ['st'])
            else:
                P.scan(hb[:, 0:n][:, ::-1], ab_a[:, 0:n][:, ::-1], ab_b[:, 0:n][:, ::-1], st[:, 0:1])
                P.cp(st[:], hb[:, 0:1], ['hb'], ['st'])
                P.tt(hsum[:, s0:s1], hsum[:, s0:s1], hb[:, 0:n], ALU.add, ['hsum', 'hb'], ['hsum'])
                P.dma(ab_a[:, 0:n], S_gg[:, s0:s1], ['S_gg'], ['ab_a'])
                P.tt(hsum[:, s0:s1], hsum[:, s0:s1], ab_a[:, 0:n], ALU.mult, ['hsum', 'ab_a'], ['hsum'])
                P.dma(out_lru[:, s0:s1], hsum[:, s0:s1], ['hsum'], ['out_lru'])
    P.finish([out_dn, out_lru])
    P.barrier()
    return P


def np_inputs(norm_g, sc, sh, sc_ctx=None, sh_ctx=None):
    if sc_ctx is None:
        return {"np_g": fm16(norm_g), "np_sc": fm16(sc)[None], "np_sh": fm16(sh)[None]}
    return {"np_g": fm16(norm_g), "np_sc": np.stack([fm16(sc), fm16(sc_ctx)]), "np_sh": np.stack([fm16(sh), fm16(sh_ctx)])}


def run_mixer_even(x_all, modv, norm_g, w_in, conv_qkv, a_log, dt_bias, dn_norm_g, lru_conv_w, lru_conv_b,
                   lru_wa, lru_ba, lru_wx, lru_bx, lam, NT, NCT):
    P = two_pass(build_mixer_even, NT, NCT)
    base = np_inputs(norm_g, modv[0, 1], modv[0, 0], modv[1, 1], modv[1, 0])
    maps = []
    for j in range(NCORES):
        s = slice(j * 128, (j + 1) * 128)
        cols_fm = np.concatenate([np.arange(j * 128, (j + 1) * 128) + o for o in (0, 1024, 2048, 4128, 5152)])
        cols_tm = np.concatenate([np.arange(3072 + j * 128, 3072 + (j + 1) * 128),
                                  np.array([4096 + j, 4096 + 8 + j, 4112 + j, 4112 + 8 + j])])
        convw = np.concatenate([conv_qkv[:, o + j * 128:o + (j + 1) * 128].T for o in (0, 1024, 2048)] + [lru_conv_w[:, s].T], axis=1)
        m = dict(base)
        m.update({
            "x_all": x_all,
            "w_fm": np.ascontiguousarray(w_in[:, cols_fm]),
            "w_tm": np.ascontiguousarray(w_in[:, cols_tm]),
            "convw": np.ascontiguousarray(convw),
            "convb": np.ascontiguousarray(lru_conv_b[s, None]),
            "lruw": np.ascontiguousarray(np.stack([lru_wa[0, j], lru_wa[1, j], lru_wx[0, j], lru_wx[1, j]])),
            "lrub": np.ascontiguousarray(np.stack([lru_ba[0, s], lru_ba[1, s], lru_bx[0, s], lru_bx[1, s]], axis=1)),
            "lam": np.ascontiguousarray(lam[:, s].T),
            "dnp": np.ascontiguousarray(np.broadcast_to(np.array([a_log[0, j], a_log[1, j], dt_bias[0, j], dt_bias[1, j]], np.float32), (128, 4))),
            "dng": np.ascontiguousarray(np.broadcast_to(dn_norm_g[None, :], (128, 128))),
        })
        maps.append(m)
    res = _run(P, maps)
    dn = np.concatenate([r["out_dn"] for r in res], axis=1)
    lru = np.concatenate([r["out_lru"].T for r in res], axis=1)
    return np.concatenate([dn, lru], axis=1)


def ffn_cast_weights(P, w_gate, w_up, w_down, Wg_s, Wu_s, Wd_s):
    with P.scope():
        stg = P.sb([128, KC, 512]); stb = P.sb([128, KC, 512], BF16)
        n = 0
        for (w, Ws) in ((w_gate, Wg_s), (w_up, Wu_s)):
            for h4 in range(HB // 4):
                P.dma(stg[:], w[:, h4 * 512:(h4 + 1) * 512].rearrange("(k p) n -> p k n", p=128))
                P.cp(stb[:], stg[:], e=('dve', 'pool', 'act')[n % 3]); n += 1
                P.dma(Ws[h4 * 4:(h4 + 1) * 4].rearrange("j p k n -> p k j n"), stb[:].rearrange("p k (j n) -> p k j n", j=4))
        sd = stg[:].rearrange("p (a b) n -> p a (b n)", a=4)
        sdb = stb[:].rearrange("p (a b) n -> p a (b n)", a=4)
        for h4 in range(HB // 4):
            P.dma(sd, w_down[h4 * 512:(h4 + 1) * 512, :].rearrange("(a p) n -> p a n", p=128))
            P.cp(sdb, sd, e=('dve', 'pool', 'act')[n % 3]); n += 1
            for dq in range(4):
                P.dma(Wd_s[dq, :, h4 * 4:(h4 + 1) * 4, :], sdb[:, :, dq * 512:(dq + 1) * 512])


def ffn_groups(P, hT_src, groups, Wg_s, Wu_s, Wd_s, epilogue):
    hTg = P.sb([128, KC, 512], BF16)
    actb = P.sb([128, HB, 512], BF16)
    wg = [P.sb([128, KC, 128], BF16) for _ in range(2)]
    wu = [P.sb([128, KC, 128], BF16) for _ in range(2)]
    wd = P.sb([128, HB, 512], BF16)
    sg = [P.sb([128, 512]) for _ in range(2)]
    pg = [P.bank(0), P.bank(1)]; pu = [P.bank(2), P.bank(3)]; po = [P.bank(4), P.bank(5)]
    it = 0
    for (tok0, TG) in groups:
        P.dma(hTg[:, :, 0:TG], hT_src[:, :, tok0:tok0 + TG].rearrange("k p n -> p k n"))
        for hb in range(HB):
            b = it % 2; it += 1
            P.dma(wg[b][:], Wg_s[hb]); P.dma(wu[b][:], Wu_s[hb])
            for kc in range(KC):
                P.mm(pg[b][:, 0:TG], wg[b][:, kc, :], hTg[:, kc, 0:TG], start=(kc == 0), stop=(kc == KC - 1))
            for kc in range(KC):
                P.mm(pu[b][:, 0:TG], wu[b][:, kc, :], hTg[:, kc, 0:TG], start=(kc == 0), stop=(kc == KC - 1))
            P.act(sg[b][:, 0:TG], pg[b][:, 0:TG], AF.Silu)
            P.tt(actb[:, hb, 0:TG], sg[b][:, 0:TG], pu[b][:, 0:TG], ALU.mult)
        n = 0
        for dq in range(4):
            P.dma(wd[:], Wd_s[dq])
            for sub in range(TG // 128):
                b = n % 2; n += 1
                for hb in range(HB):
                    P.mm(po[b][:, :], actb[:, hb, sub * 128:(sub + 1) * 128], wd[:, hb, :], start=(hb == 0), stop=(hb == HB - 1))
                epilogue(tok0 + sub * 128, dq, po[b])


def build_post_even(NTL):
    P = Prog(); nc = P.nc
    P.make_consts()
    ident = P.c['ident']
    NTOK = NTL * 128
    x_in = P.din("x_in", [NTOK, D]); a_in = P.din("a_in", [NTOK, D])
    w_out = P.din("w_out", [D, D])
    gt_bc = P.din("gt_bc", [4, 128, D])
    w_gate = P.din("w_gate", [D, FFN_H]); w_up = P.din("w_up", [D, FFN_H]); w_down = P.din("w_down", [FFN_H, D])
    x_out = P.dout("x_out", [NTOK, D])
    X_mid = P.dscr("X_mid", [NTOK, D]); H2T = P.dscr("H2T", [KC, 128, NTOK], BF16)
    Wg_s = P.dscr("Wg_s", [HB, 128, KC, 128], BF16); Wu_s = P.dscr("Wu_s", [HB, 128, KC, 128], BF16)
    Wd_s = P.dscr("Wd_s", [4, 128, HB, 512], BF16)
    npj = NormProj(P, 2)
    gtb = [P.sb([128, D]) for _ in range(4)]
    for i in range(4):
        P.dma(gtb[i][:], gt_bc[i])
    with P.scope():
        wo = load_w_bf16(P, w_out, D, 'w_out')
        at = P.sb([128, D]); aT = P.sb([128, KC, 128], BF16); xm = P.sb([128, D])
        py = [P.bank(4 + i) for i in range(4)]
        for t in range(NTL):
            cond = 1 if t == NTL - 1 else 0
            rows = slice(t * 128, (t + 1) * 128)
            P.dma(at[:], a_in[rows, :])
            P.dma(npj.xt[:], x_in[rows, :])
            for kc in range(KC):
                b, o = divmod(kc, 4)
                P.tr(npj.pT[b][:, o * 128:(o + 1) * 128], at[:, kc * 128:(kc + 1) * 128], ident[:])
            for b in range(4):
                P.cp(aT[:, b * 4:(b + 1) * 4, :], npj.pT[b][:, :].rearrange("p (g n) -> p g n", g=4), e='act')
            for dq in range(4):
                for kc in range(KC):
                    P.mm(py[dq][:, :], aT[:, kc, :], wo[:, kc, dq * 512:(dq + 1) * 512], start=(kc == 0), stop=(kc == KC - 1))
                cs = slice(dq * 512, (dq + 1) * 512)
                P.tt(xm[:, cs], py[dq][:, :], gtb[cond][:, cs], ALU.mult)
                P.tt(xm[:, cs], xm[:, cs], npj.xt[:, cs], ALU.add)
            P.dma(X_mid[rows, :], xm[:])
            npj.tile(X_mid[rows, :], cond)
            P.dma(H2T[:, :, rows].rearrange("k p n -> p k n"), npj.hT[:])
    ffn_cast_weights(P, w_gate, w_up, w_down, Wg_s, Wu_s, Wd_s)
    with P.scope():
        xm2 = [P.sb([128, 512]) for _ in range(2)]; yo = [P.sb([128, 512]) for _ in range(2)]
        cnt = [0]

        def epi(row0, dq, ps):
            b = cnt[0] % 2; cnt[0] += 1
            cond = 1 if row0 >= (NTL - 1) * 128 else 0
            cs = slice(dq * 512, (dq + 1) * 512)
            P.dma(xm2[b][:], X_mid[row0:row0 + 128, cs])
            P.tt(yo[b][:], ps[:, :], gtb[2 + cond][:, cs], ALU.mult)
            P.tt(yo[b][:], yo[b][:], xm2[b][:], ALU.add, e='pool')
            P.dma(x_out[row0:row0 + 128, cs], yo[b][:])

        groups = [(s0, min(512, NTOK - s0)) for s0 in range(0, NTOK, 512)]
        ffn_groups(P, H2T, groups, Wg_s, Wu_s, Wd_s, epi)
    P.finish([x_out])
    P.barrier()
    return P


def run_post_even(x_lat, ctx, act_all, modv, norm2_g, w_out, w_gate, w_up, w_down):
    L = x_lat.shape[0]; C = ctx.shape[0]
    lt = L // NCORES; ct = C // NCORES
    NTL = lt // 128 + 1
    P = two_pass(build_post_even, NTL)
    base = np_inputs(norm2_g, modv[0, 4], modv[0, 3], modv[1, 4], modv[1, 3])
    gt_bc = np.ascontiguousarray(np.broadcast_to(np.stack([modv[0, 2], modv[1, 2], modv[0, 5], modv[1, 5]])[:, None, :], (4, 128, D)))
    maps = []
    for j in range(NCORES):
        xi = np.zeros((NTL * 128, D), np.float32); ai = np.zeros((NTL * 128, D), np.float32)
        xi[:lt] = x_lat[j * lt:(j + 1) * lt]; xi[lt:lt + ct] = ctx[j * ct:(j + 1) * ct]
        ai[:lt] = act_all[C + j * lt:C + (j + 1) * lt]; ai[lt:lt + ct] = act_all[j * ct:(j + 1) * ct]
        m = dict(base)
        m.update({"x_in": xi, "a_in": ai, "w_out": w_out, "gt_bc": gt_bc, "w_gate": w_gate, "w_up": w_up, "w_down": w_down})
        maps.append(m)
    res = _run(P, maps)
    x1 = np.concatenate([r["x_out"][:lt] for r in res], axis=0)
    c1 = np.concatenate([r["x_out"][lt:lt + ct] for r in res], axis=0)
    return x1, c1


def build_mixer_odd(NT, NCT, phases='AC'):
    import os
    P = Prog(); nc = P.nc
    c = P.make_consts()
    ident, ones = c['ident'], c['ones']
    NTOK = NT * 128
    x_all = P.din("x_all", [NTOK, D])
    w_fm_d = P.din("w_fm", [D, 512])
    w_gd_d = P.din("w_gd", [D, 32])
    w_tm_d = P.din("w_tm", [D, 768])
    wg2_d = P.din("wg2", [2, 17, 256])
    out_o = P.dout("out_o", [NTOK, 256])
    out_sg = P.dout("out_sg", [NTOK, 256])
    S_qT = P.dscr("S_qT", [NT, 128, 2, 128]); S_kT = P.dscr("S_kT", [NT, 128, 2, 128])
    S_kv = P.dscr("S_kv", [NT, 128, 512])
    S_gd = P.dscr("S_gd", [NT, 16, 2, 128])
    S_o = P.dscr("S_o", [NT, 128, 256])

    npj = NormProj(P, 2)
    w_fm = load_w_bf16(P, w_fm_d, 512, 'w_fm')
    w_gd = load_w_bf16(P, w_gd_d, 32, 'w_gd')
    w_tm = load_w_bf16(P, w_tm_d, 768, 'w_tm')
    b4, b5, b6, b7 = P.bank(4), P.bank(5), P.bank(6), P.bank(7)
    stq = P.sb([128, 2, 128]); stk = P.sb([128, 2, 128]); stkv = P.sb([128, 512]); stsg = P.sb([128, 256]); stgd = P.sb([16, 2, 128])
    for t in (range(NT) if 'A' in phases else []):
        npj.tile(x_all[t * 128:(t + 1) * 128, :], 1 if t < NCT else 0)
        for g in range(4):
            for kc in range(KC):
                P.mm(b4[:, g * 128:(g + 1) * 128], w_fm[:, kc, g * 128:(g + 1) * 128], npj.hT[:, kc, :], start=(kc == 0), stop=(kc == KC - 1))
        if float(os.environ.get('DBGC', '99')) < 1: continue
        for d in range(2):
            for kc in range(KC):
                P.mm(b5[0:16, d * 128:(d + 1) * 128], w_gd[:, kc, d * 16:(d + 1) * 16], npj.hT[:, kc, :], start=(kc == 0), stop=(kc == KC - 1))
        if float(os.environ.get('DBGC', '99')) < 2: continue
        for kc in range(KC):
            P.mm(b6[:, :], npj.hT[:, kc, :], w_tm[:, kc, 0:512], start=(kc == 0), stop=(kc == KC - 1))
        for kc in range(KC):
            P.mm(b7[:, 0:256], npj.hT[:, kc, :], w_tm[:, kc, 512:768], start=(kc == 0), stop=(kc == KC - 1))
        if float(os.environ.get('DBGC', '99')) < 3: continue
        P.act(stq[:].rearrange("p a n -> p (a n)"), b4[:, 0:256], AF.Identity, scale=1.0 / 16.0)
        if float(os.environ.get('DBGC', '99')) < 4: continue
        P.cp(stk[:].rearrange("p a n -> p (a n)"), b4[:, 256:512], e='act')
        if float(os.environ.get('DBGC', '99')) < 5: continue
        P.cp(stgd[:].rearrange("p a n -> p (a n)"), b5[0:16, 0:256], e='act')
        if float(os.environ.get('DBGC', '99')) < 6: continue
        P.cp(stkv[:], b6[:, :], e='act')
        if float(os.environ.get('DBGC', '99')) < 7: continue
        P.act(stsg[:], b7[:, 0:256], AF.Silu)
        if float(os.environ.get('DBGC', '99')) < 8: continue
        P.dma(S_qT[t], stq[:]); P.dma(S_kT[t], stk[:]); P.dma(S_kv[t], stkv[:]); P.dma(S_gd[t], stgd[:])
        P.dma(out_sg[t * 128:(t + 1) * 128, :], stsg[:])
    P.barrier()

    wg2 = P.sb([17, 2, 256])
    P.dma(wg2[:], wg2_d.rearrange("d r n -> r d n"))
    qT = P.sb([128, 2, 128]); kT = P.sb([128, 2, 128]); kv = P.sb([128, 512]); gd = P.sb([17, 2, 128])
    P.memset(gd[:], 1.0)
    glog = P.sb([128, 256]); gcs = P.sb([128, 256]); eg = P.sb([128, 2, 128]); en = P.sb([128, 2, 128])
    qt = P.sb([128, 2, 128]); kt2 = P.sb([128, 2, 128]); kdec = P.sb([128, 256]); egl = P.sb([128, 2])
    attm = P.sb([128, 128]); S = P.sb([128, 2, 256]); o = P.sb([128, 256]); of = P.sb([128, 256])
    b0, b1, b2, b3 = P.bank(0), P.bank(1), P.bank(2), P.bank(3)
    for d in (range(2) if 'C' in phases else []):
        inc = c['inc_f'] if d == 0 else c['inc_r']
        order = list(range(NT)) if d == 0 else (list(range(NCT - 1, -1, -1)) + list(range(NT - 1, NCT - 1, -1)))
        P.memset(S[:], 0.0)
        for t in order:
            P.dma(qT[:], S_qT[t]); P.dma(kT[:], S_kT[t]); P.dma(kv[:], S_kv[t]); P.dma(gd[0:16, :, :], S_gd[t])
            P.mm(b0[:, 0:256], gd[:, d, :], wg2[:, d, :])
            P.act(glog[:], b0[:, 0:256], AF.Sigmoid)
            P.act(glog[:], glog[:], AF.Ln)
            P.ts(glog[:], glog[:], 1.0 / 16.0, None, ALU.mult, None)
            P.mm(b1[:, 0:256], inc[:], glog[:])
            P.mm(b1[:, 256:512], ones[:], glog[:])
            for h in range(2):
                P.mm(b2[:, h * 128:(h + 1) * 128], glog[:, h * 128:(h + 1) * 128], inc[:])
            for h in range(2):
                P.mm(b2[:, 256 + h:257 + h], glog[:, h * 128:(h + 1) * 128], ones[:, 0:1])
            P.cp(gcs[:], b1[:, 0:256], e='act')
            P.act(eg[:].rearrange("p a n -> p (a n)"), b2[:, 0:256], AF.Exp)
            P.act(en[:].rearrange("p a n -> p (a n)"), b2[:, 0:256], AF.Exp, scale=-1.0)
            P.act(egl[:], b2[:, 256:258], AF.Exp)
            P.tt(qt[:], qT[:], eg[:], ALU.mult)
            P.tt(kt2[:], kT[:], en[:], ALU.mult)
            P.tt(kdec[:], b1[:, 256:512], gcs[:], ALU.subtract)
            P.act(kdec[:], kdec[:], AF.Exp)
            P.tt(kdec[:], kdec[:], kv[:, 0:256], ALU.mult)
            for h in range(2):
                P.mm(b3[:, 0:128], kt2[:, h, :], qt[:, h, :], start=(h == 0), stop=(h == 1))
            P.tt(attm[:], b3[:, 0:128], inc[:], ALU.mult)
            P.mm(b4[:, 0:256], attm[:], kv[:, 256:512], start=True, stop=False)
            P.mm(b4[:, 0:256], qt[:, 0, :], S[:, 0, :], start=False, stop=False)
            P.mm(b4[:, 0:256], qt[:, 1, :], S[:, 1, :], start=False, stop=True)
            P.cp(o[:], b4[:, 0:256], e='act')
            for h in range(2):
                P.mm(b5[:, h * 256:(h + 1) * 256], kdec[:, h * 128:(h + 1) * 128], kv[:, 256:512])
            for h in range(2):
                P.stt(S[:, h, :], S[:, h, :], egl[:, h:h + 1], b5[:, h * 256:(h + 1) * 256], ALU.mult, ALU.add)
            if d == 0:
                P.dma(S_o[t], o[:])
            else:
                P.dma(of[:], S_o[t])
                P.tt(o[:], o[:], of[:], ALU.add, e='pool')
                P.dma(out_o[t * 128:(t + 1) * 128, :], o[:])
    P.finish([out_o, out_sg])
    P.barrier()
    return P


def run_mixer_odd(x_all, modv, norm_g, w_in, wg2, bg, NT, NCT):
    P = two_pass(build_mixer_odd, NT, NCT)
    base = np_inputs(norm_g, modv[0, 1], modv[0, 0], modv[1, 1], modv[1, 0])
    maps = []
    for j in range(NCORES):
        h, s = divmod(j, 2)
        qc = np.arange(h * 256, (h + 1) * 256); kc_ = 1024 + qc
        vc = 2048 + h * 512 + s * 256 + np.arange(256); gc_ = 4096 + h * 512 + s * 256 + np.arange(256)
        w2 = np.stack([np.concatenate([wg2[d][:, h * 256:(h + 1) * 256], bg[d][None, h * 256:(h + 1) * 256]], axis=0) for d in range(2)])
        m = dict(base)
        m.update({"x_all": x_all,
                  "w_fm": np.ascontiguousarray(w_in[:, np.concatenate([qc, kc_])]),
                  "w_gd": np.ascontiguousarray(w_in[:, 6144:6176]),
                  "w_tm": np.ascontiguousarray(w_in[:, np.concatenate([kc_, vc, gc_])]),
                  "wg2": np.ascontiguousarray(w2.astype(np.float32))})
        maps.append(m)
    res = _run(P, maps)
    o = np.concatenate([r["out_o"] for r in res], axis=1)
    sg = np.concatenate([r["out_sg"] for r in res], axis=1)
    return o, sg


def build_post_odd(NTL):
    P = Prog(); nc = P.nc
    P.make_consts()
    ident = P.c['ident']
    NTOK = NTL * 128
    x_in = P.din("x_in", [NTOK, D]); o_in = P.din("o_in", [NTOK, D]); sg_in = P.din("sg_in", [NTOK, D])
    w_out = P.din("w_out", [D, D])
    bc_in = P.din("bc_in", [2, 128, D])
    rw_in = P.din("rw_in", [128, KC * 8]); rb_in = P.din("rb_in", [128, 8])
    x_mid = P.dout("x_mid", [NTOK, D]); H2T = P.dout("H2T", [KC, 128, NTOK], BF16); G_out = P.dout("G_out", [128, NTL * 8])
    X_s = P.dscr("X_s", [NTOK, D])
    npj = NormProj(P, 1, keep32=True)
    gtb = P.sb([128, D]); gng = P.sb([128, D]); rw = P.sb([128, KC, 8]); rb = P.sb([128, 8])
    P.dma(gtb[:], bc_in[0]); P.dma(gng[:], bc_in[1]); P.dma(rw[:].rearrange("p k n -> p (k n)"), rw_in[:, :]); P.dma(rb[:], rb_in[:, :])
    wo = load_w_bf16(P, w_out, D, 'w_out')
    ot = P.sb([128, D]); sgt = P.sb([128, D]); aT = P.sb([128, KC, 128], BF16); xm = P.sb([128, D])
    ss4 = P.sb([128, 4]); G_all = P.sb([128, NTL, 8]); lg = P.sb([128, 8]); m8 = P.sb([128, 8]); w12 = P.sb([128, 2])
    e1 = P.sb([128, 8]); e2 = P.sb([128, 8])
    py = [P.bank(4 + i) for i in range(4)]
    for t in range(NTL):
        rows = slice(t * 128, (t + 1) * 128)
        P.dma(ot[:], o_in[rows, :]); P.dma(sgt[:], sg_in[rows, :]); P.dma(npj.xt[:], x_in[rows, :])
        for h in range(4):
            P.act(npj.junk[:, h * 512:(h + 1) * 512], ot[:, h * 512:(h + 1) * 512], AF.Square, accum_out=ss4[:, h:h + 1])
        P.ts(ss4[:], ss4[:], 1.0 / 512, EPS, ALU.mult, ALU.add)
        P.act(ss4[:], ss4[:], AF.Sqrt)
        P.recip(ss4[:], ss4[:])
        for h in range(4):
            P.stt(ot[:, h * 512:(h + 1) * 512], ot[:, h * 512:(h + 1) * 512], ss4[:, h:h + 1], gng[:, h * 512:(h + 1) * 512], ALU.mult, ALU.mult)
        P.tt(ot[:], ot[:], sgt[:], ALU.mult, e='pool')
        for kc in range(KC):
            b, o = divmod(kc, 4)
            P.tr(npj.pT[b][:, o * 128:(o + 1) * 128], ot[:, kc * 128:(kc + 1) * 128], ident[:])
        for b in range(4):
            P.cp(aT[:, b * 4:(b + 1) * 4, :], npj.pT[b][:, :].rearrange("p (g n) -> p g n", g=4), e='act')
        for dq in range(4):
            for kc in range(KC):
                P.mm(py[dq][:, :], aT[:, kc, :], wo[:, kc, dq * 512:(dq + 1) * 512], start=(kc == 0), stop=(kc == KC - 1))
            cs = slice(dq * 512, (dq + 1) * 512)
            P.tt(xm[:, cs], py[dq][:, :], gtb[:, cs], ALU.mult)
            P.tt(xm[:, cs], xm[:, cs], npj.xt[:, cs], ALU.add)
        P.dma(X_s[rows, :], xm[:]); P.dma(x_mid[rows, :], xm[:])
        npj.tile(X_s[rows, :], 0)
        P.dma(H2T[:, :, rows].rearrange("k p n -> p k n"), npj.hT[:])
        for kc in range(KC):
            P.mm(py[0][:, 0:8], npj.hT32[:, kc, :], rw[:, kc, :], start=(kc == 0), stop=(kc == KC - 1))
        P.tt(lg[:], py[0][:, 0:8], rb[:], ALU.add)
        r_, w_ = P._rw([lg[:]], [m8[:]])
        P.op('dve', r_, w_, lambda: nc.vector.max(out=m8[:], in_=lg[:]))
        P.tt(w12[:, 0:1], m8[:, 0:1], m8[:, 1:2], ALU.subtract)
        P.act(w12[:, 0:1], w12[:, 0:1], AF.Sigmoid)
        P.ts(w12[:, 1:2], w12[:, 0:1], -1.0, 1.0, ALU.mult, ALU.add)
        P.ts(e1[:], lg[:], m8[:, 0:1], w12[:, 0:1], ALU.is_equal, ALU.mult)
        P.ts(e2[:], lg[:], m8[:, 1:2], w12[:, 1:2], ALU.is_equal, ALU.mult)
        P.tt(G_all[:, t, :], e1[:], e2[:], ALU.add)
    P.dma(G_out[:, :], G_all[:].rearrange("p t n -> p (t n)"))
    P.finish([x_mid, H2T, G_out])
    P.barrier()
    return P


def run_post_odd(x1, o, sg, modv, gla_norm_g, norm2_g, w_out, router_w, router_b):
    L = x1.shape[0]; lt = L // NCORES; NTL = lt // 128
    P = two_pass(build_post_odd, NTL)
    base = np_inputs(norm2_g, modv[0, 4], modv[0, 3])
    bc = np.ascontiguousarray(np.broadcast_to(np.stack([modv[0, 2], np.tile(gla_norm_g, 4)])[:, None, :], (2, 128, D)))
    rw = np.ascontiguousarray(router_w.reshape(KC, 128, 8).transpose(1, 0, 2).reshape(128, KC * 8))
    rb = np.ascontiguousarray(np.broadcast_to(router_b[None, :], (128, 8)))
    maps = []
    for j in range(NCORES):
        sl = slice(j * lt, (j + 1) * lt)
        m = dict(base)
        m.update({"x_in": np.ascontiguousarray(x1[sl]), "o_in": np.ascontiguousarray(o[sl]), "sg_in": np.ascontiguousarray(sg[sl]),
                  "w_out": w_out, "bc_in": bc, "rw_in": rw, "rb_in": rb})
        maps.append(m)
    res = _run(P, maps)
    x_mid = np.concatenate([r["x_mid"] for r in res], axis=0)
    H2T = np.concatenate([r["H2T"] for r in res], axis=2)
    G = np.concatenate([r["G_out"].reshape(128, NTL, 8).transpose(1, 0, 2).reshape(lt, 8) for r in res], axis=0)
    return x_mid, H2T, G


def build_expert(NTOK):
    P = Prog(); nc = P.nc
    NT = NTOK // 128
    H2T = P.din("H2T", [KC, 128, NTOK], BF16)
    gate = P.din("gate", [128, NT])
    w_gate = P.din("w_gate", [D, FFN_H]); w_up = P.din("w_up", [D, FFN_H]); w_down = P.din("w_down", [FFN_H, D])
    y = P.dout("y", [NTOK, D])
    Wg_s = P.dscr("Wg_s", [HB, 128, KC, 128], BF16); Wu_s = P.dscr("Wu_s", [HB, 128, KC, 128], BF16)
    Wd_s = P.dscr("Wd_s", [4, 128, HB, 512], BF16)
    gt = P.sb([128, NT])
    P.dma(gt[:], gate[:, :])
    ffn_cast_weights(P, w_gate, w_up, w_down, Wg_s, Wu_s, Wd_s)
    yo = [P.sb([128, 512]) for _ in range(2)]
    cnt = [0]

    def epi(row0, dq, ps):
        b = cnt[0] % 2; cnt[0] += 1
        t = row0 // 128
        P.act(yo[b][:], ps[:, :], AF.Identity, scale=gt[:, t:t + 1])
        P.dma(y[row0:row0 + 128, dq * 512:(dq + 1) * 512], yo[b][:])

    groups = [(s0, min(512, NTOK - s0)) for s0 in range(0, NTOK, 512)]
    ffn_groups(P, H2T, groups, Wg_s, Wu_s, Wd_s, epi)
    P.finish([y])
    P.barrier()
    return P


def run_experts(H2T, G, w_gate, w_up, w_down):
    L = G.shape[0]
    P = two_pass(build_expert, L)
    maps = []
    for e in range(NCORES):
        maps.append({"H2T": H2T, "gate": np.ascontiguousarray(G[:, e].reshape(L // 128, 128).T),
                     "w_gate": w_gate[e], "w_up": w_up[e], "w_down": w_down[e]})
    res = _run(P, maps)
    return [r["y"] for r in res]


def build_final(NTL):
    P = Prog(); nc = P.nc
    NTOK = NTL * 128
    x_mid = P.din("x_mid", [NTOK, D]); parts = P.din("parts", [NCORES, NTOK, D]); bc_in = P.din("bc_in", [2, 128, D])
    out = P.dout("out", [NTOK, D])
    gtb = P.sb([128, D]); fg = P.sb([128, D])
    P.dma(gtb[:], bc_in[0]); P.dma(fg[:], bc_in[1])
    xm = P.sb([128, D]); pt = [P.sb([128, D]) for _ in range(NCORES)]; junk = P.sb([128, D], BF16); ss = P.sb([128, 1])
    for t in range(NTL):
        rows = slice(t * 128, (t + 1) * 128)
        P.dma(xm[:], x_mid[rows, :])
        for e in range(NCORES):
            P.dma(pt[e][:], parts[e, rows, :])
        for (a, b, eng) in ((0, 1, 'dve'), (2, 3, 'pool'), (4, 5, 'dve'), (6, 7, 'pool'), (0, 2, 'dve'), (4, 6, 'pool'), (0, 4, 'dve')):
            P.tt(pt[a][:], pt[a][:], pt[b][:], ALU.add, e=eng)
        P.tt(pt[0][:], pt[0][:], gtb[:], ALU.mult)
        P.tt(xm[:], xm[:], pt[0][:], ALU.add, e='pool')
        P.act(junk[:], xm[:], AF.Square, accum_out=ss[:])
        P.ts(ss[:], ss[:], 1.0 / D, EPS, ALU.mult, ALU.add)
        P.act(ss[:], ss[:], AF.Sqrt)
        P.recip(ss[:], ss[:])
        P.stt(xm[:], xm[:], ss[:, 0:1], fg[:], ALU.mult, ALU.mult)
        P.dma(out[rows, :], xm[:])
    P.finish([out])
    P.barrier()
    return P


def run_final(x_mid, parts, gt2, final_g):
    L = x_mid.shape[0]; lt = L // NCORES; NTL = lt // 128
    P = two_pass(build_final, NTL)
    bc = np.ascontiguousarray(np.broadcast_to(np.stack([gt2, final_g])[:, None, :], (2, 128, D)))
    maps = []
    for j in range(NCORES):
        sl = slice(j * lt, (j + 1) * lt)
        maps.append({"x_mid": np.ascontiguousarray(x_mid[sl]), "parts": np.ascontiguousarray(np.stack([p[sl] for p in parts])), "bc_in": bc})
    res = _run(P, maps)
    return np.concatenate([r["out"] for r in res], axis=0)


def kernel(x, c, ctx, c_ctx, mod_w, mod_b, norm1_g, norm2_g, ev_w_in, ev_conv_qkv, ev_dn_a_log, ev_dn_dt_bias,
           ev_dn_norm_g, ev_lru_conv_w, ev_lru_conv_b, ev_lru_wa, ev_lru_ba, ev_lru_wx, ev_lru_bx, ev_lru_lambda,
           ev_w_out, ev_ffn_w_gate, ev_ffn_w_up, ev_ffn_w_down, od_w_in, od_gla_wg2, od_gla_bg, od_gla_norm_g,
           od_w_out, od_router_w, od_router_b, od_exp_w_gate, od_exp_w_up, od_exp_w_down, final_norm_g):
    f = lambda a: np.asarray(a, dtype=np.float32)
    x = f(x)[0]; ctx = f(ctx)[0]
    L = x.shape[0]; C = ctx.shape[0]
    NCT = C // 128; NT = NCT + L // 128
    modv = run_mod(f(c), f(c_ctx), f(mod_w), f(mod_b))
    x_all = np.concatenate([ctx, x], axis=0)
    act = run_mixer_even(x_all, modv[0], f(norm1_g)[0], f(ev_w_in)[0], f(ev_conv_qkv)[0], f(ev_dn_a_log)[0], f(ev_dn_dt_bias)[0],
                         f(ev_dn_norm_g)[0], f(ev_lru_conv_w)[0], f(ev_lru_conv_b)[0], f(ev_lru_wa)[0], f(ev_lru_ba)[0],
                         f(ev_lru_wx)[0], f(ev_lru_bx)[0], f(ev_lru_lambda)[0], NT, NCT)
    x1, c1 = run_post_even(x, ctx, act, modv[0], f(norm2_g)[0], f(ev_w_out)[0], f(ev_ffn_w_gate)[0], f(ev_ffn_w_up)[0], f(ev_ffn_w_down)[0])
    rows = L // GRID_W
    xr = x1.reshape(rows, GRID_W, D).swapaxes(0, 1).reshape(L, D)
    x_all = np.concatenate([c1, xr], axis=0)
    o, sg = run_mixer_odd(x_all, modv[1], f(norm1_g)[1], f(od_w_in)[0], f(od_gla_wg2)[0], f(od_gla_bg)[0], NT, NCT)
    unr = lambda a: a[C:].reshape(GRID_W, rows, D).swapaxes(0, 1).reshape(L, D)
    x_mid, H2T, G = run_post_odd(x1, unr(o), unr(sg), modv[1], f(od_gla_norm_g)[0], f(norm2_g)[1], f(od_w_out)[0],
                                 f(od_router_w)[0], f(od_router_b)[0])
    parts = run_experts(H2T, G, f(od_exp_w_gate)[0], f(od_exp_w_up)[0], f(od_exp_w_down)[0])
    out = run_final(x_mid, parts, modv[1][0, 5], f(final_norm_g))
    return out[None].astype(np.float32)
```

```python
import numpy as np
import concourse.bass as bass
import concourse.mybir as mybir
from concourse.bass_utils import run_bass_kernel_spmd

F32 = mybir.dt.float32
BF16 = mybir.dt.bfloat16
AF = mybir.ActivationFunctionType
ALU = mybir.AluOpType

D = 2048
KC = 16
FFN_H = 5632
HB = 44
EPS = 1e-6
NCORES = 8
CTX = 256
GRID_W = 64


class Prog:
    CAP = 30000
    DCAP = 1800
    NDS = 8

    def __init__(self):
        self.nc = bass.Bass("TRN2", target_bir_lowering=False)
        nc = self.nc
        self.eng = {'pe': nc.tensor, 'dve': nc.vector, 'act': nc.scalar, 'pool': nc.gpsimd, 'sp': nc.sync}
        self.sems = {k: [] for k in self.eng}
        self.cnt = {k: 0 for k in self.eng}
        self.dsems = {}
        self.dcnt = {}
        self.dn = {k: 0 for k in self.eng}
        self.waited = {k: {} for k in self.eng}
        self.last_w = {}
        self.reads = {}
        self.nsem = 0
        self.ninstr = 0
        self.nalloc = 0
        self.used = set()
        self.needed = Prog.NEEDED

    def sb(self, shape, dtype=F32, name=None):
        self.nalloc += 1
        st = getattr(self, '_stack', None)
        if st is not None:
            return st.enter_context(self.nc.sbuf_tensor(name or f"sb{self.nalloc}", list(shape), dtype))
        return self.nc.alloc_sbuf_tensor(name or f"sb{self.nalloc}", list(shape), dtype)

    def scope(self):
        import contextlib
        prog = self

        @contextlib.contextmanager
        def cm():
            st = contextlib.ExitStack()
            prev = getattr(prog, '_stack', None)
            prog._stack = st
            try:
                yield
            finally:
                prog.barrier()
                prog._stack = prev
                st.close()
        return cm()

    def ps(self, shape, dtype=F32, name=None):
        self.nalloc += 1
        return self.nc.alloc_psum_tensor(name or f"ps{self.nalloc}", list(shape), dtype)

    def din(self, name, shape, dtype=F32):
        return self.nc.dram_tensor(name, list(shape), dtype, kind="ExternalInput").ap()

    def dout(self, name, shape, dtype=F32):
        return self.nc.dram_tensor(name, list(shape), dtype, kind="ExternalOutput").ap()

    def dscr(self, name, shape, dtype=F32):
        return self.nc.dram_tensor(name, list(shape), dtype, kind="Internal").ap()

    NEEDED = None

    def _newsem(self, nm):
        self.nsem += 1
        return self.nc.alloc_semaphore(name=f"{nm}_{self.nsem}")

    def _csem(self, e, rank):
        ep, v = divmod(rank, self.CAP)
        while len(self.sems[e]) <= ep:
            self.sems[e].append(self._newsem(e))
        return self.sems[e][ep], v + 1

    def _resolve(self, ticket):
        if ticket[0] == 'd':
            return ticket[1], ticket[2]
        _, e, idx = ticket
        self.used.add((e, idx))
        if self.needed is None:
            rank = idx
        else:
            rank = self.needed[e][idx]
        return self._csem(e, rank)

    def _wait_many(self, e, tickets):
        cbest = {}
        dbest = {}
        for t in tickets:
            if t is None:
                continue
            if t[0] == 'c':
                if e == 'pe' and t[1] == 'pe':
                    continue
                if cbest.get(t[1], -1) < t[2]:
                    cbest[t[1]] = t[2]
            else:
                k = id(t[1])
                if k not in dbest or dbest[k][1] < t[2]:
                    dbest[k] = (t[1], t[2])
        for e2, idx in cbest.items():
            if self.waited[e].get(e2, -1) >= idx:
                continue
            sem, val = self._resolve(('c', e2, idx))
            self.eng[e].wait_ge(sem, val)
            self.waited[e][e2] = idx
        for k, (sem, val) in dbest.items():
            if self.waited[e].get(k, 0) >= val:
                continue
            self.eng[e].wait_ge(sem, val)
            self.waited[e][k] = val

    def _wait(self, e, ticket):
        self._wait_many(e, [ticket])

    def _deps(self, e, reads, writes):
        ts = [self.last_w.get(r) for r in list(reads) + list(writes)]
        for w in writes:
            ts.extend(self.reads.get(w, []))
        self._wait_many(e, ts)

    def _record(self, ticket, reads, writes):
        for r in reads:
            self.reads.setdefault(r, []).append(ticket)
        for w in writes:
            self.last_w[w] = ticket
            self.reads[w] = []

    def op(self, e, reads, writes, fn):
        self._deps(e, reads, writes)
        idx = self.cnt[e]
        ins = fn()
        if self.needed is None:
            sem, val = self._csem(e, idx)
            ins.then_inc(sem, 1)
        elif idx in self.needed[e]:
            sem, val = self._csem(e, self.needed[e][idx])
            ins.then_inc(sem, 1)
        self.cnt[e] = idx + 1
        self.ninstr += 1
        t = ('c', e, idx)
        self._record(t, reads, writes)
        return t

    def dma(self, out, in_, reads=None, writes=None, q='sp', **kw):
        reads, writes = self._rw([in_], [out])
        self._deps(q, reads, writes)
        n = self.dn[q]
        self.dn[q] = n + 1
        key = (q, n % self.NDS)
        c = self.dcnt.get(key, 0)
        ep, v = divmod(c, self.DCAP)
        lst = self.dsems.setdefault(key, [])
        while len(lst) <= ep:
            lst.append(self._newsem('d' + q))
        sem = lst[ep]
        if v > 0:
            self._wait(q, ('d', sem, 16 * v))
        elif ep > 0:
            self._wait(q, ('d', lst[ep - 1], 16 * self.DCAP))
        self.eng[q].dma_start(out=out, in_=in_, **kw).then_inc(sem, 16)
        self.dcnt[key] = c + 1
        self.ninstr += 1
        t = ('d', sem, 16 * (v + 1))
        self._record(t, reads, writes)
        return t

    def finish(self, outs):
        for r in outs:
            for k in ([r] if isinstance(r, str) else self._nm(r)):
                self._wait('sp', self.last_w.get(k))

    def barrier(self):
        tickets = []
        for e2 in self.eng:
            if self.cnt[e2] > 0:
                tickets.append(('c', e2, self.cnt[e2] - 1))
        for key, c in self.dcnt.items():
            if c > 0:
                ep, v = divmod(c - 1, self.DCAP)
                tickets.append(('d', self.dsems[key][ep], 16 * (v + 1)))
        for e in self.eng:
            self._wait_many(e, [t for t in tickets if not (t[0] == 'c' and t[1] == e)])

    def bank(self, i):
        if not hasattr(self, '_banks'):
            self._banks = [self.ps([128, 512], name=f"bank{j}") for j in range(8)]
        return self._banks[i]

    def _nm(self, x):
        try:
            name = x.tensor.name
        except AttributeError:
            return []
        sp = getattr(self, 'split', {}).get(name)
        if sp is None:
            return [name]
        row, blk = sp
        dims = list(x.ap)[1:]
        lo = x.offset % row
        hi = lo
        for st, n in dims:
            if st < 0:
                lo += st * (n - 1)
            else:
                hi += st * (n - 1)
        return [f"{name}#{b}" for b in range(lo // blk, hi // blk + 1)]

    def _rw(self, ins, outs):
        r = [n for a in ins for n in self._nm(a)]
        w = [n for a in outs for n in self._nm(a)]
        for a in ins:
            try:
                if type(a.tensor).__name__.startswith('PSum'):
                    w.extend(self._nm(a))
            except AttributeError:
                pass
        return r, w

    def mm(self, out, lhsT, rhs, reads=None, writes=None, start=True, stop=True):
        nc = self.nc
        r, w = self._rw([lhsT, rhs], [out])
        return self.op('pe', r, w, lambda: nc.tensor.matmul(out, lhsT, rhs, start=start, stop=stop))

    def tr(self, out, in_, ident, reads=None, writes=None):
        nc = self.nc
        r, w = self._rw([in_, ident], [out])
        return self.op('pe', r, w, lambda: nc.tensor.transpose(out, in_, ident))

    def act(self, out, in_, func, reads=None, writes=None, **kw):
        nc = self.nc
        r, w = self._rw([in_, kw.get('scale'), kw.get('bias')], [out, kw.get('accum_out')])
        return self.op('act', r, w, lambda: nc.scalar.activation(out=out, in_=in_, func=func, **kw))

    def ts(self, out, in0, s1, s2, op0, op1, reads=None, writes=None, e='dve'):
        eng = self.eng[e]
        r, w = self._rw([in0, s1, s2], [out])
        if op1 is None:
            return self.op(e, r, w, lambda: eng.tensor_scalar(out=out, in0=in0, scalar1=s1, scalar2=None, op0=op0))
        return self.op(e, r, w, lambda: eng.tensor_scalar(out=out, in0=in0, scalar1=s1, scalar2=s2, op0=op0, op1=op1))

    def tt(self, out, in0, in1, op, reads=None, writes=None, e='dve'):
        eng = self.eng[e]
        r, w = self._rw([in0, in1], [out])
        return self.op(e, r, w, lambda: eng.tensor_tensor(out=out, in0=in0, in1=in1, op=op))

    def stt(self, out, in0, scalar, in1, op0, op1, reads=None, writes=None):
        nc = self.nc
        r, w = self._rw([in0, scalar, in1], [out])
        return self.op('dve', r, w, lambda: nc.vector.scalar_tensor_tensor(out=out, in0=in0, scalar=scalar, in1=in1, op0=op0, op1=op1))

    def cp(self, out, in_, reads=None, writes=None, e='dve'):
        eng = self.eng[e]
        if e == 'act':
            return self.act(out, in_, AF.Copy)
        r, w = self._rw([in_], [out])
        return self.op(e, r, w, lambda: eng.tensor_copy(out=out, in_=in_))

    def memset(self, ap, val, writes=None, e='pool'):
        eng = self.eng[e]
        r, w = self._rw([], [ap])
        return self.op(e, r, w, lambda: eng.memset(ap, val))

    def recip(self, out, in_, reads=None, writes=None):
        nc = self.nc
        r, w = self._rw([in_], [out])
        return self.op('dve', r, w, lambda: nc.vector.reciprocal(out=out, in_=in_))

    def scan(self, out, d0, d1, init):
        nc = self.nc
        r, w = self._rw([d0, d1, init], [out])
        return self.op('dve', r, w, lambda: nc.vector.tensor_tensor_scan(out=out, data0=d0, data1=d1, initial=init, op0=ALU.mult, op1=ALU.add))

    def asel(self, t, pattern, cmp, fill, cm):
        nc = self.nc
        r, w = self._rw([t], [t])
        return self.op('pool', r, w, lambda: nc.gpsimd.affine_select(out=t, in_=t, pattern=pattern, compare_op=cmp, fill=fill, base=0, channel_multiplier=cm))

    def make_consts(self):
        nc = self.nc
        c = {}
        ident = self.sb([128, 128]); c['ident'] = ident
        self.memset(ident[:], 1.0, ['ident'])
        self.asel(ident[:], [[1, 128]], ALU.is_equal, 0.0, -1)
        ones = self.sb([128, 128]); c['ones'] = ones
        self.memset(ones[:], 1.0, ['ones'])
        def tri(name, fillv, keepv, cmp, cm, st):
            t = self.sb([128, 128]); c[name] = t
            self.memset(t[:], keepv, [name])
            self.asel(t[:], [[st, 128]], cmp, fillv, cm)
        tri('inc_f', 0.0, 1.0, ALU.is_ge, -1, 1)
        tri('inc_r', 0.0, 1.0, ALU.is_ge, 1, -1)
        NEG = -30000.0
        tri('neg_f', NEG, 0.0, ALU.is_ge, -1, 1)
        tri('neg_r', NEG, 0.0, ALU.is_ge, 1, -1)
        tri('negs_f', NEG, 0.0, ALU.is_gt, -1, 1)
        tri('negs_r', NEG, 0.0, ALU.is_gt, 1, -1)
        self.c = c
        self.barrier()
        return c


def two_pass(build, *args, **kw):
    Prog.NEEDED = None
    P1 = build(*args, **kw)
    needed = {e: {} for e in P1.eng}
    for (e, idx) in sorted(P1.used):
        needed[e][idx] = len(needed[e])
    Prog.NEEDED = needed
    try:
        P2 = build(*args, **kw)
    finally:
        Prog.NEEDED = None
    return P2


def _run(P, in_maps):
    res = run_bass_kernel_spmd(P.nc, in_maps, core_ids=list(range(len(in_maps))))
    return res.results


def fm16(v):
    return np.ascontiguousarray(np.asarray(v, np.float32).reshape(KC, 128).T)


def build_mod(depth, ncol):
    P = Prog(); nc = P.nc
    condT = P.din("condT", [128, KC * 2])
    w = P.din("w", [depth, D, ncol])
    b = P.din("b", [depth, 2, ncol])
    out = P.dout("out", [depth, 2, ncol])
    sc = P.sb([128, KC * 2])
    P.dma(sc[:], condT[:, :], [], ['sc'])
    P.act(sc[:], sc[:], AF.Silu, ['sc'], ['sc'])
    wt = P.sb([128, KC, ncol])
    bt = P.sb([2, ncol])
    ot = P.sb([2, ncol])
    nps = (ncol + 511) // 512
    pss = [P.ps([2, 512]) for _ in range(nps)]
    for l in range(depth):
        P.dma(wt[:], w[l].rearrange("(k p) n -> p k n", p=128), ['wt_free'], ['wt'])
        P.dma(bt[:], b[l], [], ['bt'])
        for g in range(nps):
            n0 = g * 512; n1 = min(ncol, n0 + 512)
            for kc in range(KC):
                P.mm(pss[g][:, 0:n1 - n0], sc[:, kc * 2:kc * 2 + 2], wt[:, kc, n0:n1], ['sc', 'wt'], [f'mps{g}'],
                     start=(kc == 0), stop=(kc == KC - 1))
            P.tt(ot[:, n0:n1], pss[g][:, 0:n1 - n0], bt[:, n0:n1], ALU.add, [f'mps{g}', 'bt'], ['ot'])
        P.dma(out[l], ot[:], ['ot'], ['out'])
    P.finish([out])
    return P


def run_mod(c, c_ctx, mod_w, mod_b):
    depth = mod_w.shape[0]
    ncol = mod_w.shape[2] // NCORES
    cond = np.stack([np.asarray(c).reshape(-1), np.asarray(c_ctx).reshape(-1)], 0)
    condT = np.ascontiguousarray(cond.reshape(2, KC, 128).transpose(2, 1, 0).reshape(128, KC * 2))
    P = two_pass(build_mod, depth, ncol)
    maps = []
    for j in range(NCORES):
        maps.append({"condT": condT,
                     "w": np.ascontiguousarray(mod_w[:, :, j * ncol:(j + 1) * ncol]),
                     "b": np.ascontiguousarray(np.broadcast_to(mod_b[:, None, j * ncol:(j + 1) * ncol], (depth, 2, ncol)))})
    res = _run(P, maps)
    m = np.concatenate([r["out"] for r in res], axis=2)
    return m.reshape(depth, 2, 6, D)


class NormProj:
    def __init__(self, P, ncond, keep32=False):
        self.P = P
        self.keep32 = keep32
        self.hT32 = P.sb([128, KC, 128]) if keep32 else None
        if keep32:
            if not hasattr(P, 'split'):
                P.split = {}
            P.split[self.hT32.name] = (KC * 128, 128)
        nc = P.nc
        self.xt = P.sb([128, D]); self.xn = P.sb([128, D]); self.junk = P.sb([128, D], BF16)
        self.ss = P.sb([128, 1]); self.rs = P.sb([128, 1])
        self.hT = P.sb([128, KC, 128], BF16)
        if not hasattr(P, 'split'):
            P.split = {}
        P.split[self.hT.name] = (KC * 128, 128)
        self.pT = [P.bank(i) for i in range(4)]
        self.g_in = P.din("np_g", [128, KC])
        self.sc_in = P.din("np_sc", [ncond, 128, KC])
        self.sh_in = P.din("np_sh", [ncond, 128, KC])
        self.ncond = ncond
        gt = P.sb([128, KC])
        self.msc = [P.sb([128, KC]) for _ in range(ncond)]
        self.msh = [P.sb([128, KC]) for _ in range(ncond)]
        P.dma(gt[:], self.g_in[:, :], [], ['np_gt'])
        for c in range(ncond):
            P.dma(self.msc[c][:], self.sc_in[c], [], [f'np_msc{c}'])
            P.dma(self.msh[c][:], self.sh_in[c], [], [f'np_msh{c}'])
            P.stt(self.msc[c][:], self.msc[c][:], 1.0, gt[:], ALU.add, ALU.mult, ['np_gt', f'np_msc{c}'], [f'np_msc{c}'])

    def tile(self, x_rows, cond):
        P = self.P; nc = P.nc
        P.dma(self.xt[:], x_rows, [], ['np_xt'])
        ss, rs = self.ss, self.rs
        P.act(self.junk[:], self.xt[:], AF.Square, ['np_xt'], ['np_junk', 'np_ss'], accum_out=ss[:])
        P.ts(rs[:], ss[:], 1.0 / D, EPS, ALU.mult, ALU.add, ['np_ss'], ['np_rs'])
        P.act(rs[:], rs[:], AF.Sqrt, ['np_rs'], ['np_rs'])
        P.recip(rs[:], rs[:], ['np_rs'], ['np_rs'])
        P.act(self.xn[:], self.xt[:], AF.Identity, ['np_xt', 'np_rs'], ['np_xn'], scale=rs[:, 0:1])
        ident = P.c['ident']
        for kc in range(KC):
            b, o = divmod(kc, 4)
            P.tr(self.pT[b][:, o * 128:(o + 1) * 128], self.xn[:, kc * 128:(kc + 1) * 128], ident[:], ['np_xn', 'ident'], [f'np_pT{b}'])
        msc, msh = self.msc[cond], self.msh[cond]
        dst = self.hT32 if self.keep32 else self.hT
        for kc in range(KC):
            b, o = divmod(kc, 4)
            rd = [f'np_pT{b}', f'np_msc{cond}', f'np_msh{cond}']
            if b % 2 == 0:
                P.ts(dst[:, kc, :], self.pT[b][:, o * 128:(o + 1) * 128], msc[:, kc:kc + 1], msh[:, kc:kc + 1],
                     ALU.mult, ALU.add, rd, [f'hT{kc}'])
            else:
                P.act(dst[:, kc, :], self.pT[b][:, o * 128:(o + 1) * 128], AF.Identity, rd, [f'hT{kc}'],
                      scale=msc[:, kc:kc + 1], bias=msh[:, kc:kc + 1])
        if self.keep32:
            for b in range(4):
                P.cp(self.hT[:, b * 4:(b + 1) * 4, :], self.hT32[:, b * 4:(b + 1) * 4, :], e=('pool' if b % 2 else 'act'))


def load_w_bf16(P, w_dram, ncols, name):
    wt = P.sb([128, KC, ncols], BF16)
    stg = P.sb([128, 4, ncols])
    for q in range(KC // 4):
        P.dma(stg[:], w_dram[q * 512:(q + 1) * 512, :].rearrange("(k p) n -> p k n", p=128), [], [name + '_stg'])
        P.cp(wt[:, q * 4:(q + 1) * 4, :], stg[:], [name + '_stg'], [name], e=('dve' if q % 2 == 0 else 'pool'))
    return wt


def build_mixer_even(NT, NCT, phases='ABCD'):
    P = Prog(); nc = P.nc
    c = P.make_consts()
    ident, ones = c['ident'], c['ones']
    NTOK = NT * 128
    x_all = P.din("x_all", [NTOK, D])
    w_fm_d = P.din("w_fm", [D, 640])
    w_tm_d = P.din("w_tm", [D, 132])
    convw_d = P.din("convw", [128, 16])
    convb_d = P.din("convb", [128, 1])
    lruw_d = P.din("lruw", [4, 128, 128])
    lrub_d = P.din("lrub", [128, 4])
    lam_d = P.din("lam", [128, 2])
    dnp_d = P.din("dnp", [128, 4])
    dng_d = P.din("dng", [128, 128])
    out_dn = P.dout("out_dn", [NTOK, 128])
    out_lru = P.dout("out_lru", [128, NTOK])

    COFF = 4; LOFF = 4 + NCT * 128 + 4
    NPAD = LOFF + (NT - NCT) * 128 + 4
    S_fm = P.dscr("S_fm", [5, 128, NPAD])
    S_z = P.dscr("S_z", [NTOK, 128])
    S_ab = P.dscr("S_ab", [NTOK, 4])
    S_qT = P.dscr("S_qT", [NT, 128, 128]); S_kT = P.dscr("S_kT", [NT, 128, 128])
    S_k = P.dscr("S_k", [NT, 128, 128]); S_v = P.dscr("S_v", [NT, 128, 128])
    S_g = P.dscr("S_g", [NTOK, 4])
    S_a = P.dscr("S_a", [2, 128, NTOK]); S_b = P.dscr("S_b", [2, 128, NTOK]); S_gg = P.dscr("S_gg", [128, NTOK])
    S_o = P.dscr("S_o", [NTOK, 128])

    def col0(t):
        return (COFF + t * 128) if t < NCT else (LOFF + (t - NCT) * 128)

    zt = P.sb([128, 5, 4])
    P.memset(zt[:], 0.0, ['zt'])
    for off in (0, COFF + NCT * 128, LOFF + (NT - NCT) * 128):
        P.dma(S_fm[:, :, off:off + 4].rearrange("g p n -> p g n"), zt[:], ['zt'], ['S_fm'])

    npj = NormProj(P, 2)
    w_fm = load_w_bf16(P, w_fm_d, 640, 'w_fm')
    w_tm = load_w_bf16(P, w_tm_d, 132, 'w_tm')
    pA = [P.bank(4), P.bank(5)]
    stgA = P.sb([128, 5, 128]); stgZ = P.sb([128, 128]); ab_all = P.sb([128, NT, 4]); g_all = P.sb([128, NT, 4])
    for t in (range(NT) if 'A' in phases else []):
        npj.tile(x_all[t * 128:(t + 1) * 128, :], 1 if t < NCT else 0)
        import os
        DBG = int(os.environ.get('DBG', '9'))
        if DBG < 2: continue
        for g in range(5):
            b, o = divmod(g, 4)
            for kc in range(KC):
                P.mm(pA[b][:, o * 128:(o + 1) * 128], w_fm[:, kc, g * 128:(g + 1) * 128], npj.hT[:, kc, :], ['w_fm', f'hT{kc}'], [f'pA{b}'],
                     start=(kc == 0), stop=(kc == KC - 1))
        if DBG < 3: continue
        for kc in range(KC):
            P.mm(pA[1][:, 128:260], npj.hT[:, kc, :], w_tm[:, kc, :], ['w_tm', f'hT{kc}'], ['pA1'], start=(kc == 0), stop=(kc == KC - 1))
        if DBG < 4: continue
        P.cp(stgA[:, 0:4, :], pA[0][:, :].rearrange("p (g n) -> p g n", g=4), ['pA0'], ['stgA'])
        P.cp(stgA[:, 4, :], pA[1][:, 0:128], ['pA1'], ['stgA'], e='act')
        P.act(stgZ[:], pA[1][:, 128:256], AF.Silu, ['pA1'], ['stgZ'])
        P.cp(ab_all[:, t, :], pA[1][:, 256:260])
        if DBG < 5: continue
        c0 = col0(t)
        M5 = os.environ.get('M5', 'abc')
        if 'a' in M5: P.dma(S_fm[:, :, c0:c0 + 128].rearrange("g p n -> p g n"), stgA[:], ['stgA'], ['S_fm'])
        if 'b' in M5: P.dma(S_z[t * 128:(t + 1) * 128, :], stgZ[:], ['stgZ'], ['S_z'])


    convw = P.sb([128, 16]); convb = P.sb([128, 1]); lruw = P.sb([128, 4, 128]); lrub = P.sb([128, 4])
    lam = P.sb([128, 2]); dnp = P.sb([128, 4]); dng = P.sb([128, 128])
    P.dma(convw[:], convw_d[:, :], [], ['convw']); P.dma(convb[:], convb_d[:, :], [], ['convb'])
    P.dma(lruw[:], lruw_d.rearrange("g p n -> p g n"), [], ['lruw']); P.dma(lrub[:], lrub_d[:, :], [], ['lrub'])
    P.dma(lam[:], lam_d[:, :], [], ['lam']); P.dma(dnp[:], dnp_d[:, :], [], ['dnp']); P.dma(dng[:], dng_d[:, :], [], ['dng'])
    nsp8 = P.sb([128, 2]); nea = P.sb([128, 2])
    P.act(nsp8[:], lam[:], AF.Exp, ['lam'], ['nsp8'], scale=-1.0)
    P.act(nsp8[:], nsp8[:], AF.Ln, ['nsp8'], ['nsp8'], bias=1.0)
    P.ts(nsp8[:], nsp8[:], -8.0, None, ALU.mult, None, ['nsp8'], ['nsp8'])
    P.act(nea[:], dnp[:, 0:2], AF.Exp, ['dnp'], ['nea'])
    P.ts(nea[:], nea[:], -1.0, None, ALU.mult, None, ['nea'], ['nea'])

    pre = P.sb([128, 4, 131]); gr = P.sb([128, 128]); cv = P.sb([128, 4, 128]); sq = P.sb([128, 256]); rn = P.sb([128, 256])
    qk = P.sb([128, 2, 128]); ktm = P.sb([128, 128]); vtm = P.sb([128, 128])
    pB = P.bank(6); pB2 = P.bank(5)
    gate = P.sb([128, 4, 128]); av = P.sb([128, 2, 128]); bv = P.sb([128, 2, 128]); tmp = P.sb([128, 2, 128]); gg = P.sb([128, 128])
    for t in (range(NT) if 'B' in phases else []):
        c0 = col0(t)
        P.dma(pre[:], S_fm[0:4, :, c0 - 2:c0 + 129].rearrange("g p n -> p g n"), ['S_fm'], ['pre'])
        P.dma(gr[:], S_fm[4, :, c0:c0 + 128], ['S_fm'], ['gr'])
        for g in range(4):
            P.ts(cv[:, g, :], pre[:, g, 0:128], convw[:, g * 4:g * 4 + 1], None, ALU.mult, None, ['pre', 'convw'], ['cv'])
            for j in range(1, 4):
                P.stt(cv[:, g, :], pre[:, g, j:j + 128], convw[:, g * 4 + j:g * 4 + j + 1], cv[:, g, :], ALU.mult, ALU.add,
                      ['pre', 'convw', 'cv'], ['cv'])
        if float(os.environ.get('DBGB', '9')) < 1: continue
        P.act(cv[:, 0:3, :], cv[:, 0:3, :], AF.Silu, ['cv'], ['cv'])
        if float(os.environ.get('DBGB', '9')) < 1.2: continue
        P.act(sq[:], cv[:, 0:2, :].rearrange("p g n -> p (g n)"), AF.Square, ['cv'], ['sq'])
        if float(os.environ.get('DBGB', '9')) < 1.4: continue
        P.mm(pB[:, 0:256], ones[:], sq[:], ['ones', 'sq'], ['pB'])
        if float(os.environ.get('DBGB', '9')) < 1.5: continue
        P.ts(rn[:], pB[:, 0:256], EPS, None, ALU.add, None, ['pB'], ['rn'])
        if float(os.environ.get('DBGB', '9')) < 1.6: continue
        P.act(rn[:], rn[:], AF.Sqrt, ['rn'], ['rn'])
        P.recip(rn[:], rn[:], ['rn'], ['rn'])
        if float(os.environ.get('DBGB', '9')) < 1.8: continue
        P.stt(qk[:, 0, :], rn[:, 0:128], 128.0 ** -0.5, cv[:, 0, :], ALU.mult, ALU.mult, ['rn', 'cv'], ['qk'])
        P.tt(qk[:, 1, :], rn[:, 128:256], cv[:, 1, :], ALU.mult, ['rn', 'cv'], ['qk'])
        if float(os.environ.get('DBGB', '9')) < 2: continue
        P.tr(pB2[:, 0:128], qk[:, 1, :], ident[:], ['qk', 'ident'], ['pB2'])
        if float(os.environ.get('DBGB', '9')) < 2.2: continue
        P.tr(pB2[:, 128:256], cv[:, 2, :], ident[:], ['cv', 'ident'], ['pB2'])
        if float(os.environ.get('DBGB', '9')) < 2.4: continue
        if os.environ.get('KE', 'act') == 'ts':
            P.ts(ktm[:], pB2[:, 0:128], 1.0, None, ALU.mult, None)
        else:
            P.cp(ktm[:], pB2[:, 0:128], e=os.environ.get('KE', 'act'))
        if float(os.environ.get('DBGB', '9')) < 2.6: continue
        P.cp(vtm[:], pB2[:, 128:256], ['pB2'], ['vtm'], e='act')
        if float(os.environ.get('DBGB', '9')) < 3: continue
        P.dma(S_qT[t], qk[:, 0, :], ['qk'], ['S_qT']); P.dma(S_kT[t], qk[:, 1, :], ['qk'], ['S_kT'])
        P.dma(S_k[t], ktm[:], ['ktm'], ['S_k']); P.dma(S_v[t], vtm[:], ['vtm'], ['S_v'])
        if float(os.environ.get('DBGB', '9')) < 4: continue
        P.ts(cv[:, 3, :], cv[:, 3, :], convb[:, 0:1], None, ALU.add, None, ['cv', 'convb'], ['cv'])
        for g in range(4):
            P.mm(pB[:, 256:384] if g % 2 == 0 else pB[:, 384:512], lruw[:, g, :], cv[:, 3, :], ['lruw', 'cv'], [f'pBg{g % 2}'])
            P.act(gate[:, g, :], pB[:, 256:384] if g % 2 == 0 else pB[:, 384:512], AF.Sigmoid, [f'pBg{g % 2}', 'lrub'], ['gate'],
                  bias=lrub[:, g:g + 1])
        for d in range(2):
            P.act(av[:, d, :], gate[:, d, :], AF.Exp, ['gate', 'nsp8'], ['av'], scale=nsp8[:, d:d + 1])
        P.tt(tmp[:], av[:], av[:], ALU.mult, ['av'], ['tmp'])
        P.ts(tmp[:], tmp[:], -1.0, 1.0, ALU.mult, ALU.add, ['tmp'], ['tmp'])
        P.act(tmp[:], tmp[:], AF.Sqrt, ['tmp'], ['tmp'])
        P.tt(bv[:], tmp[:], gate[:, 2:4, :], ALU.mult, ['tmp', 'gate'], ['bv'])
        for d in range(2):
            P.tt(bv[:, d, :], bv[:, d, :], cv[:, 3, :], ALU.mult, ['bv', 'cv'], ['bv'])
        P.act(gg[:], gr[:], AF.Square, ['gr'], ['gg'])
        P.ts(gg[:], gg[:], 0.044715, 1.0, ALU.mult, ALU.add, ['gg'], ['gg'])
        P.tt(gg[:], gg[:], gr[:], ALU.mult, ['gg', 'gr'], ['gg'])
        P.act(gg[:], gg[:], AF.Sigmoid, ['gg'], ['gg'], scale=1.5957691216057308)
        P.tt(gg[:], gg[:], gr[:], ALU.mult, ['gg', 'gr'], ['gg'])
        if float(os.environ.get('DBGB', '9')) < 5: continue
        P.dma(S_a[:, :, t * 128:(t + 1) * 128].rearrange("d p n -> p d n"), av[:], ['av'], ['S_a'])
        P.dma(S_b[:, :, t * 128:(t + 1) * 128].rearrange("d p n -> p d n"), bv[:], ['bv'], ['S_b'])
        P.dma(S_gg[:, t * 128:(t + 1) * 128], gg[:], ['gg'], ['S_gg'])
        if float(os.environ.get('DBGB', '9')) < 6: continue
        ab = ab_all[:, t, :]; gl4 = g_all[:, t, :]
        P.tt(gl4[:, 0:2], ab[:, 0:2], dnp[:, 2:4], ALU.add, ['ab', 'dnp'], ['gl4'])
        P.ts(gl4[:, 2:4], ab[:, 2:4], -1.0, None, ALU.mult, None, ['ab'], ['gl4'])
        P.act(gl4, gl4, AF.Exp)
        P.act(gl4, gl4, AF.Ln, bias=1.0)
        P.tt(gl4[:, 0:2], gl4[:, 0:2], nea[:], ALU.mult, ['gl4', 'nea'], ['gl4'])
        P.ts(gl4[:, 2:4], gl4[:, 2:4], -1.0, None, ALU.mult, None, ['gl4'], ['gl4'])

    P.barrier()
    S_ob = P.dscr("S_ob", [NTOK, 128])
    one_row = ones[0:1, :]

    def mkbufs(d):
        B = {}
        for nm in ('qT', 'kT', 'kt', 'vt', 'S', 'EAT', 'EA', 'EQT', 'QKm', 'WT', 'vnew', 'kdec', 'otmp', 'o', 'N0', 'N1', 'NT0', 'NT1'):
            B[nm] = P.sb([128, 128])
        B['g4'] = P.sb([128, 4]); B['rows'] = P.sb([1, 3, 128]); B['cols'] = P.sb([128, 6]); B['glb'] = P.sb([128, 1])
        B['Y'] = P.sb([128, 256])
        k = 4 * d
        B['p_E'] = P.bank(k); B['p_col'] = P.bank(k)[:, 384:386]
        B['p_KK'] = P.bank(k + 1); B['p_Y'] = P.bank(k + 2); B['p_N'] = P.bank(k + 3); B['p_row'] = P.bank(k + 3)[0:1, 384:512]
        return B

    def chunk(d, t, B):
        inc = c['inc_f'] if d == 0 else c['inc_r']
        neg = c['neg_f'] if d == 0 else c['neg_r']
        negs = c['negs_f'] if d == 0 else c['negs_r']
        negsT = c['negs_r'] if d == 0 else c['negs_f']
        qT, kT, kt, vt, g4, S = B['qT'], B['kT'], B['kt'], B['vt'], B['g4'], B['S']
        rows, cols, glb, EAT, EA, EQT = B['rows'], B['cols'], B['glb'], B['EAT'], B['EA'], B['EQT']
        Nb = [B['N0'], B['N1']]; NTb = [B['NT0'], B['NT1']]
        QKm, Y, WT, vnew, kdec, otmp, o = B['QKm'], B['Y'], B['WT'], B['vnew'], B['kdec'], B['otmp'], B['o']
        p_row, p_col, p_E, p_KK, p_Y, p_N = B['p_row'], B['p_col'], B['p_E'], B['p_KK'], B['p_Y'], B['p_N']
        P.dma(qT[:], S_qT[t]); P.dma(kT[:], S_kT[t]); P.dma(kt[:], S_k[t]); P.dma(vt[:], S_v[t])
        P.cp(g4[:], g_all[:, t, :], e='pool')
        gcol = g4[:, d:d + 1]; lbcol = g4[:, 2 + d:3 + d]
        P.mm(p_row, gcol, inc[:])
        P.cp(rows[:, 0, :], p_row, e='act')
        P.act(rows[:, 1, :], p_row, AF.Identity, scale=-1.0)
        P.mm(p_row, gcol, inc[:], start=True, stop=False)
        P.mm(p_row, lbcol, ident[:], start=False, stop=True)
        P.cp(rows[:, 2, :], p_row, e='act')
        P.mm(p_col[:, 0:1], inc[:], gcol)
        P.mm(p_col[:, 1:2], ones[:], gcol)
        P.cp(cols[:, 5:6], p_col[:, 0:1], e='act')
        P.cp(glb[:], p_col[:, 1:2], e='act')
        P.act(cols[:, 0:1], lbcol, AF.Exp)
        P.act(cols[:, 1:2], cols[:, 5:6], AF.Exp, bias=lbcol)
        P.act(cols[:, 2:3], cols[:, 5:6], AF.Exp)
        P.act(cols[:, 3:4], cols[:, 5:6], AF.Exp, scale=-1.0, bias=glb[:, 0:1])
        P.act(cols[:, 4:5], glb[:], AF.Exp)
        P.mm(p_E[:, 0:128], one_row, rows[:, 2, :], start=True, stop=False)
        P.mm(p_E[:, 0:128], rows[:, 1, :], one_row, start=False, stop=False)
        P.mm(p_E[:, 0:128], ident[:], negs[:], start=False, stop=True)
        P.mm(p_E[:, 128:256], rows[:, 2, :], one_row, start=True, stop=False)
        P.mm(p_E[:, 128:256], one_row, rows[:, 1, :], start=False, stop=False)
        P.mm(p_E[:, 128:256], ident[:], negsT[:], start=False, stop=True)
        P.mm(p_E[:, 256:384], one_row, rows[:, 0, :], start=True, stop=False)
        P.mm(p_E[:, 256:384], rows[:, 1, :], one_row, start=False, stop=False)
        P.mm(p_E[:, 256:384], ident[:], neg[:], start=False, stop=True)
        P.act(EAT[:], p_E[:, 0:128], AF.Exp)
        P.act(EA[:], p_E[:, 128:256], AF.Exp)
        P.act(EQT[:], p_E[:, 256:384], AF.Exp)
        P.mm(p_KK[:, 0:128], kT[:], kT[:])
        P.mm(p_KK[:, 128:256], kT[:], qT[:])
        P.stt(NTb[0][:], p_KK[:, 0:128], -1.0, EAT[:], ALU.mult, ALU.mult)
        P.stt(Nb[0][:], p_KK[:, 0:128], -1.0, EA[:], ALU.mult, ALU.mult)
        P.tt(QKm[:], p_KK[:, 128:256], EQT[:], ALU.mult)
        P.ts(Y[:, 0:128], vt[:], cols[:, 0:1], None, ALU.mult, None)
        P.ts(Y[:, 128:256], kt[:], cols[:, 1:2], None, ALU.mult, None, e='pool')
        for l in range(7):
            a, b_ = l % 2, (l + 1) % 2
            P.mm(p_Y[:, 0:256], NTb[a][:], Y[:])
            if l < 6:
                P.mm(p_N[:, 0:128], NTb[a][:], Nb[a][:])
                P.mm(p_N[:, 128:256], Nb[a][:], NTb[a][:])
            P.tt(Y[:], p_Y[:, 0:256], Y[:], ALU.add)
            if l < 6:
                P.cp(Nb[b_][:], p_N[:, 0:128], e='act')
                P.cp(NTb[b_][:], p_N[:, 128:256], e='act')
        P.tr(p_N[:, 256:384], Y[:, 128:256], ident[:])
        P.cp(WT[:], p_N[:, 256:384], e='act')
        P.mm(p_Y[:, 256:384], WT[:], S[:])
        P.tt(vnew[:], Y[:, 0:128], p_Y[:, 256:384], ALU.subtract)
        P.mm(p_KK[:, 256:384], qT[:], S[:])
        P.mm(p_KK[:, 384:512], QKm[:], vnew[:])
        P.cp(otmp[:], p_KK[:, 384:512], e='act')
        P.stt(o[:], p_KK[:, 256:384], cols[:, 2:3], otmp[:], ALU.mult, ALU.add)
        P.ts(kdec[:], kt[:], cols[:, 3:4], None, ALU.mult, None, e='pool')
        P.mm(p_Y[:, 384:512], kdec[:], vnew[:])
        P.stt(S[:], S[:], cols[:, 4:5], p_Y[:, 384:512], ALU.mult, ALU.add)
        P.dma((S_o if d == 0 else S_ob)[t * 128:(t + 1) * 128, :], o[:])

    if 'C' in phases:
        BF = [mkbufs(0), mkbufs(1)]
        orders = [list(range(NT)), list(range(NCT - 1, -1, -1)) + list(range(NT - 1, NCT - 1, -1))]
        for d in range(2):
            P.memset(BF[d]['S'][:], 0.0)
        for i in range(NT):
            for d in range(2):
                chunk(d, orders[d][i], BF[d])
        of = P.sb([128, 128]); ob = P.sb([128, 128]); zs = P.sb([128, 128]); oss = P.sb([128, 1]); ojunk = P.sb([128, 128])
        for t in range(NT):
            rws = slice(t * 128, (t + 1) * 128)
            P.dma(of[:], S_o[rws, :]); P.dma(ob[:], S_ob[rws, :]); P.dma(zs[:], S_z[rws, :])
            P.tt(of[:], of[:], ob[:], ALU.add)
            P.act(ojunk[:], of[:], AF.Square, accum_out=oss[:])
            P.ts(oss[:], oss[:], 1.0 / 128, EPS, ALU.mult, ALU.add)
            P.act(oss[:], oss[:], AF.Sqrt)
            P.recip(oss[:], oss[:])
            P.stt(of[:], of[:], oss[:, 0:1], dng[:], ALU.mult, ALU.mult)
            P.tt(of[:], of[:], zs[:], ALU.mult, e='pool')
            P.dma(out_dn[rws, :], of[:])
    P.barrier()

    BL = min(2048, (NT - NCT) * 128)
    hsum = P.sb([128, NTOK]); ab_a = P.sb([128, BL]); ab_b = P.sb([128, BL]); hb = P.sb([128, BL]); st = P.sb([128, 1])
    segs = [(0, NCT * 128)] + [(s, min(s + BL, NTOK)) for s in range(NCT * 128, NTOK, BL)]
    for d in (range(2) if 'D' in phases else []):
        sl = segs if d == 0 else ([segs[0]] + segs[1:][::-1])
        P.memset(st[:], 0.0, ['st'])
        for (s0, s1) in sl:
            n = s1 - s0
            P.dma(ab_a[:, 0:n], S_a[d, :, s0:s1], ['S_a'], ['ab_a'])
            P.dma(ab_b[:, 0:n], S_b[d, :, s0:s1], ['S_b'], ['ab_b'])
            if d == 0:
                P.scan(hsum[:, s0:s1], ab_a[:, 0:n], ab_b[:, 0:n], st[:, 0:1])
                P.cp(st[:], hsum[:, s1 - 1:s1], ['hsum'], ['st'])
            else:
                P.scan(hb[:, 0:n][:, ::-1], ab_a[:, 0:n][:, ::-1], ab_b[:, 0:n][:, ::-1], st[:, 0:1])
                P.cp(st[:], hb[:, 0:1], ['hb'], ['st'])
                P.tt(hsum[:, s0:s1], hsum[:, s0:s1], hb[:, 0:n], ALU.add, ['hsum', 'hb'], ['hsum'])
                P.dma(ab_a[:, 0:n], S_gg[:, s0:s1], ['S_gg'], ['ab_a'])
                P.tt(hsum[:, s0:s1], hsum[:, s0:s1], ab_a[:, 0:n], ALU.mult, ['hsum', 'ab_a'], ['hsum'])
                P.dma(out_lru[:, s0:s1], hsum[:, s0:s1], ['hsum'], ['out_lru'])
    P.finish([out_dn, out_lru])
    P.barrier()
    return P


def np_inputs(norm_g, sc, sh, sc_ctx=None, sh_ctx=None):
    if sc_ctx is None:
        return {"np_g": fm16(norm_g), "np_sc": fm16(sc)[None], "np_sh": fm16(sh)[None]}
    return {"np_g": fm16(norm_g), "np_sc": np.stack([fm16(sc), fm16(sc_ctx)]), "np_sh": np.stack([fm16(sh), fm16(sh_ctx)])}


def run_mixer_even(x_all, modv, norm_g, w_in, conv_qkv, a_log, dt_bias, dn_norm_g, lru_conv_w, lru_conv_b,
                   lru_wa, lru_ba, lru_wx, lru_bx, lam, NT, NCT):
    P = two_pass(build_mixer_even, NT, NCT)
    base = np_inputs(norm_g, modv[0, 1], modv[0, 0], modv[1, 1], modv[1, 0])
    maps = []
    for j in range(NCORES):
        s = slice(j * 128, (j + 1) * 128)
        cols_fm = np.concatenate([np.arange(j * 128, (j + 1) * 128) + o for o in (0, 1024, 2048, 4128, 5152)])
        cols_tm = np.concatenate([np.arange(3072 + j * 128, 3072 + (j + 1) * 128),
                                  np.array([4096 + j, 4096 + 8 + j, 4112 + j, 4112 + 8 + j])])
        convw = np.concatenate([conv_qkv[:, o + j * 128:o + (j + 1) * 128].T for o in (0, 1024, 2048)] + [lru_conv_w[:, s].T], axis=1)
        m = dict(base)
        m.update({
            "x_all": x_all,
            "w_fm": np.ascontiguousarray(w_in[:, cols_fm]),
            "w_tm": np.ascontiguousarray(w_in[:, cols_tm]),
            "convw": np.ascontiguousarray(convw),
            "convb": np.ascontiguousarray(lru_conv_b[s, None]),
            "lruw": np.ascontiguousarray(np.stack([lru_wa[0, j], lru_wa[1, j], lru_wx[0, j], lru_wx[1, j]])),
            "lrub": np.ascontiguousarray(np.stack([lru_ba[0, s], lru_ba[1, s], lru_bx[0, s], lru_bx[1, s]], axis=1)),
            "lam": np.ascontiguousarray(lam[:, s].T),
            "dnp": np.ascontiguousarray(np.broadcast_to(np.array([a_log[0, j], a_log[1, j], dt_bias[0, j], dt_bias[1, j]], np.float32), (128, 4))),
            "dng": np.ascontiguousarray(np.broadcast_to(dn_norm_g[None, :], (128, 128))),
        })
        maps.append(m)
    res = _run(P, maps)
    dn = np.concatenate([r["out_dn"] for r in res], axis=1)
    lru = np.concatenate([r["out_lru"].T for r in res], axis=1)
    return np.concatenate([dn, lru], axis=1)


def ffn_cast_weights(P, w_gate, w_up, w_down, Wg_s, Wu_s, Wd_s):
    with P.scope():
        stg = P.sb([128, KC, 512]); stb = P.sb([128, KC, 512], BF16)
        n = 0
        for (w, Ws) in ((w_gate, Wg_s), (w_up, Wu_s)):
            for h4 in range(HB // 4):
                P.dma(stg[:], w[:, h4 * 512:(h4 + 1) * 512].rearrange("(k p) n -> p k n", p=128))
                P.cp(stb[:], stg[:], e=('dve', 'pool', 'act')[n % 3]); n += 1
                P.dma(Ws[h4 * 4:(h4 + 1) * 4].rearrange("j p k n -> p k j n"), stb[:].rearrange("p k (j n) -> p k j n", j=4))
        sd = stg[:].rearrange("p (a b) n -> p a (b n)", a=4)
        sdb = stb[:].rearrange("p (a b) n -> p a (b n)", a=4)
        for h4 in range(HB // 4):
            P.dma(sd, w_down[h4 * 512:(h4 + 1) * 512, :].rearrange("(a p) n -> p a n", p=128))
            P.cp(sdb, sd, e=('dve', 'pool', 'act')[n % 3]); n += 1
            for dq in range(4):
                P.dma(Wd_s[dq, :, h4 * 4:(h4 + 1) * 4, :], sdb[:, :, dq * 512:(dq + 1) * 512])


def ffn_groups(P, hT_src, groups, Wg_s, Wu_s, Wd_s, epilogue):
    hTg = P.sb([128, KC, 512], BF16)
    actb = P.sb([128, HB, 512], BF16)
    wg = [P.sb([128, KC, 128], BF16) for _ in range(2)]
    wu = [P.sb([128, KC, 128], BF16) for _ in range(2)]
    wd = P.sb([128, HB, 512], BF16)
    sg = [P.sb([128, 512]) for _ in range(2)]
    pg = [P.bank(0), P.bank(1)]; pu = [P.bank(2), P.bank(3)]; po = [P.bank(4), P.bank(5)]
    it = 0
    for (tok0, TG) in groups:
        P.dma(hTg[:, :, 0:TG], hT_src[:, :, tok0:tok0 + TG].rearrange("k p n -> p k n"))
        for hb in range(HB):
            b = it % 2; it += 1
            P.dma(wg[b][:], Wg_s[hb]); P.dma(wu[b][:], Wu_s[hb])
            for kc in range(KC):
                P.mm(pg[b][:, 0:TG], wg[b][:, kc, :], hTg[:, kc, 0:TG], start=(kc == 0), stop=(kc == KC - 1))
            for kc in range(KC):
                P.mm(pu[b][:, 0:TG], wu[b][:, kc, :], hTg[:, kc, 0:TG], start=(kc == 0), stop=(kc == KC - 1))
            P.act(sg[b][:, 0:TG], pg[b][:, 0:TG], AF.Silu)
            P.tt(actb[:, hb, 0:TG], sg[b][:, 0:TG], pu[b][:, 0:TG], ALU.mult)
        n = 0
        for dq in range(4):
            P.dma(wd[:], Wd_s[dq])
            for sub in range(TG // 128):
                b = n % 2; n += 1
                for hb in range(HB):
                    P.mm(po[b][:, :], actb[:, hb, sub * 128:(sub + 1) * 128], wd[:, hb, :], start=(hb == 0), stop=(hb == HB - 1))
                epilogue(tok0 + sub * 128, dq, po[b])


def build_post_even(NTL):
    P = Prog(); nc = P.nc
    P.make_consts()
    ident = P.c['ident']
    NTOK = NTL * 128
    x_in = P.din("x_in", [NTOK, D]); a_in = P.din("a_in", [NTOK, D])
    w_out = P.din("w_out", [D, D])
    gt_bc = P.din("gt_bc", [4, 128, D])
    w_gate = P.din("w_gate", [D, FFN_H]); w_up = P.din("w_up", [D, FFN_H]); w_down = P.din("w_down", [FFN_H, D])
    x_out = P.dout("x_out", [NTOK, D])
    X_mid = P.dscr("X_mid", [NTOK, D]); H2T = P.dscr("H2T", [KC, 128, NTOK], BF16)
    Wg_s = P.dscr("Wg_s", [HB, 128, KC, 128], BF16); Wu_s = P.dscr("Wu_s", [HB, 128, KC, 128], BF16)
    Wd_s = P.dscr("Wd_s", [4, 128, HB, 512], BF16)
    npj = NormProj(P, 2)
    gtb = [P.sb([128, D]) for _ in range(4)]
    for i in range(4):
        P.dma(gtb[i][:], gt_bc[i])
    with P.scope():
        wo = load_w_bf16(P, w_out, D, 'w_out')
        at = P.sb([128, D]); aT = P.sb([128, KC, 128], BF16); xm = P.sb([128, D])
        py = [P.bank(4 + i) for i in range(4)]
        for t in range(NTL):
            cond = 1 if t == NTL - 1 else 0
            rows = slice(t * 128, (t + 1) * 128)
            P.dma(at[:], a_in[rows, :])
            P.dma(npj.xt[:], x_in[rows, :])
            for kc in range(KC):
                b, o = divmod(kc, 4)
                P.tr(npj.pT[b][:, o * 128:(o + 1) * 128], at[:, kc * 128:(kc + 1) * 128], ident[:])
            for b in range(4):
                P.cp(aT[:, b * 4:(b + 1) * 4, :], npj.pT[b][:, :].rearrange("p (g n) -> p g n", g=4), e='act')
            for dq in range(4):
                for kc in range(KC):
                    P.mm(py[dq][:, :], aT[:, kc, :], wo[:, kc, dq * 512:(dq + 1) * 512], start=(kc == 0), stop=(kc == KC - 1))
                cs = slice(dq * 512, (dq + 1) * 512)
                P.tt(xm[:, cs], py[dq][:, :], gtb[cond][:, cs], ALU.mult)
                P.tt(xm[:, cs], xm[:, cs], npj.xt[:, cs], ALU.add)
            P.dma(X_mid[rows, :], xm[:])
            npj.tile(X_mid[rows, :], cond)
            P.dma(H2T[:, :, rows].rearrange("k p n -> p k n"), npj.hT[:])
    ffn_cast_weights(P, w_gate, w_up, w_down, Wg_s, Wu_s, Wd_s)
    with P.scope():
        xm2 = [P.sb([128, 512]) for _ in range(2)]; yo = [P.sb([128, 512]) for _ in range(2)]
        cnt = [0]

        def epi(row0, dq, ps):
            b = cnt[0] % 2; cnt[0] += 1
            cond = 1 if row0 >= (NTL - 1) * 128 else 0
            cs = slice(dq * 512, (dq + 1) * 512)
            P.dma(xm2[b][:], X_mid[row0:row0 + 128, cs])
            P.tt(yo[b][:], ps[:, :], gtb[2 + cond][:, cs], ALU.mult)
            P.tt(yo[b][:], yo[b][:], xm2[b][:], ALU.add, e='pool')
            P.dma(x_out[row0:row0 + 128, cs], yo[b][:])

        groups = [(s0, min(512, NTOK - s0)) for s0 in range(0, NTOK, 512)]
        ffn_groups(P, H2T, groups, Wg_s, Wu_s, Wd_s, epi)
    P.finish([x_out])
    P.barrier()
    return P


def run_post_even(x_lat, ctx, act_all, modv, norm2_g, w_out, w_gate, w_up, w_down):
    L = x_lat.shape[0]; C = ctx.shape[0]
    lt = L // NCORES; ct = C // NCORES
    NTL = lt // 128 + 1
    P = two_pass(build_post_even, NTL)
    base = np_inputs(norm2_g, modv[0, 4], modv[0, 3], modv[1, 4], modv[1, 3])
    gt_bc = np.ascontiguousarray(np.broadcast_to(np.stack([modv[0, 2], modv[1, 2], modv[0, 5], modv[1, 5]])[:, None, :], (4, 128, D)))
    maps = []
    for j in range(NCORES):
        xi = np.zeros((NTL * 128, D), np.float32); ai = np.zeros((NTL * 128, D), np.float32)
        xi[:lt] = x_lat[j * lt:(j + 1) * lt]; xi[lt:lt + ct] = ctx[j * ct:(j + 1) * ct]
        ai[:lt] = act_all[C + j * lt:C + (j + 1) * lt]; ai[lt:lt + ct] = act_all[j * ct:(j + 1) * ct]
        m = dict(base)
        m.update({"x_in": xi, "a_in": ai, "w_out": w_out, "gt_bc": gt_bc, "w_gate": w_gate, "w_up": w_up, "w_down": w_down})
        maps.append(m)
    res = _run(P, maps)
    x1 = np.concatenate([r["x_out"][:lt] for r in res], axis=0)
    c1 = np.concatenate([r["x_out"][lt:lt + ct] for r in res], axis=0)
    return x1, c1


def build_mixer_odd(NT, NCT, phases='AC'):
    import os
    P = Prog(); nc = P.nc
    c = P.make_consts()
    ident, ones = c['ident'], c['ones']
    NTOK = NT * 128
    x_all = P.din("x_all", [NTOK, D])
    w_fm_d = P.din("w_fm", [D, 512])
    w_gd_d = P.din("w_gd", [D, 32])
    w_tm_d = P.din("w_tm", [D, 768])
    wg2_d = P.din("wg2", [2, 17, 256])
    out_o = P.dout("out_o", [NTOK, 256])
    out_sg = P.dout("out_sg", [NTOK, 256])
    S_qT = P.dscr("S_qT", [NT, 128, 2, 128]); S_kT = P.dscr("S_kT", [NT, 128, 2, 128])
    S_kv = P.dscr("S_kv", [NT, 128, 512])
    S_gd = P.dscr("S_gd", [NT, 16, 2, 128])
    S_o = P.dscr("S_o", [NT, 128, 256])

    npj = NormProj(P, 2)
    w_fm = load_w_bf16(P, w_fm_d, 512, 'w_fm')
    w_gd = load_w_bf16(P, w_gd_d, 32, 'w_gd')
    w_tm = load_w_bf16(P, w_tm_d, 768, 'w_tm')
    b4, b5, b6, b7 = P.bank(4), P.bank(5), P.bank(6), P.bank(7)
    stq = P.sb([128, 2, 128]); stk = P.sb([128, 2, 128]); stkv = P.sb([128, 512]); stsg = P.sb([128, 256]); stgd = P.sb([16, 2, 128])
    for t in (range(NT) if 'A' in phases else []):
        npj.tile(x_all[t * 128:(t + 1) * 128, :], 1 if t < NCT else 0)
        for g in range(4):
            for kc in range(KC):
                P.mm(b4[:, g * 128:(g + 1) * 128], w_fm[:, kc, g * 128:(g + 1) * 128], npj.hT[:, kc, :], start=(kc == 0), stop=(kc == KC - 1))
        if float(os.environ.get('DBGC', '99')) < 1: continue
        for d in range(2):
            for kc in range(KC):
                P.mm(b5[0:16, d * 128:(d + 1) * 128], w_gd[:, kc, d * 16:(d + 1) * 16], npj.hT[:, kc, :], start=(kc == 0), stop=(kc == KC - 1))
        if float(os.environ.get('DBGC', '99')) < 2: continue
        for kc in range(KC):
            P.mm(b6[:, :], npj.hT[:, kc, :], w_tm[:, kc, 0:512], start=(kc == 0), stop=(kc == KC - 1))
        for kc in range(KC):
            P.mm(b7[:, 0:256], npj.hT[:, kc, :], w_tm[:, kc, 512:768], start=(kc == 0), stop=(kc == KC - 1))
        if float(os.environ.get('DBGC', '99')) < 3: continue
        P.act(stq[:].rearrange("p a n -> p (a n)"), b4[:, 0:256], AF.Identity, scale=1.0 / 16.0)
        if float(os.environ.get('DBGC', '99')) < 4: continue
        P.cp(stk[:].rearrange("p a n -> p (a n)"), b4[:, 256:512], e='act')
        if float(os.environ.get('DBGC', '99')) < 5: continue
        P.cp(stgd[:].rearrange("p a n -> p (a n)"), b5[0:16, 0:256], e='act')
        if float(os.environ.get('DBGC', '99')) < 6: continue
        P.cp(stkv[:], b6[:, :], e='act')
        if float(os.environ.get('DBGC', '99')) < 7: continue
        P.act(stsg[:], b7[:, 0:256], AF.Silu)
        if float(os.environ.get('DBGC', '99')) < 8: continue
        P.dma(S_qT[t], stq[:]); P.dma(S_kT[t], stk[:]); P.dma(S_kv[t], stkv[:]); P.dma(S_gd[t], stgd[:])
        P.dma(out_sg[t * 128:(t + 1) * 128, :], stsg[:])
    P.barrier()

    wg2 = P.sb([17, 2, 256])
    P.dma(wg2[:], wg2_d.rearrange("d r n -> r d n"))
    S_ob = P.dscr("S_ob", [NT, 128, 256])

    def mkbufs(d):
        B = {'qT': P.sb([128, 2, 128]), 'kT': P.sb([128, 2, 128]), 'kv': P.sb([128, 512]), 'gd': P.sb([17, 2, 128]),
             'glog': P.sb([128, 256]), 'gcs': P.sb([128, 256]), 'eg': P.sb([128, 2, 128]), 'en': P.sb([128, 2, 128]),
             'qt': P.sb([128, 2, 128]), 'kt2': P.sb([128, 2, 128]), 'kdec': P.sb([128, 256]), 'egl': P.sb([128, 2]),
             'attm': P.sb([128, 128]), 'S': P.sb([128, 2, 256]), 'o': P.sb([128, 256])}
        P.memset(B['gd'][:], 1.0)
        P.memset(B['S'][:], 0.0)
        k = 4 * d
        B['bA'] = P.bank(k); B['bB'] = P.bank(k + 1); B['bC'] = P.bank(k + 2); B['bD'] = P.bank(k + 3)
        return B

    def chunk(d, t, B):
        inc = c['inc_f'] if d == 0 else c['inc_r']
        qT, kT, kv, gd, glog, gcs, eg, en = B['qT'], B['kT'], B['kv'], B['gd'], B['glog'], B['gcs'], B['eg'], B['en']
        qt, kt2, kdec, egl, attm, S, o = B['qt'], B['kt2'], B['kdec'], B['egl'], B['attm'], B['S'], B['o']
        bA, bB, bC, bD = B['bA'], B['bB'], B['bC'], B['bD']
        P.dma(qT[:], S_qT[t]); P.dma(kT[:], S_kT[t]); P.dma(kv[:], S_kv[t]); P.dma(gd[0:16, :, :], S_gd[t])
        P.mm(bA[:, 0:256], gd[:, d, :], wg2[:, d, :])
        P.act(glog[:], bA[:, 0:256], AF.Sigmoid)
        P.act(glog[:], glog[:], AF.Ln)
        P.ts(glog[:], glog[:], 1.0 / 16.0, None, ALU.mult, None)
        P.mm(bB[:, 0:256], inc[:], glog[:])
        P.mm(bB[:, 256:512], ones[:], glog[:])
        for h in range(2):
            P.mm(bC[:, h * 128:(h + 1) * 128], glog[:, h * 128:(h + 1) * 128], inc[:])
        for h in range(2):
            P.mm(bC[:, 256 + h:257 + h], glog[:, h * 128:(h + 1) * 128], ones[:, 0:1])
        P.cp(gcs[:], bB[:, 0:256], e='act')
        P.act(eg[:].rearrange("p a n -> p (a n)"), bC[:, 0:256], AF.Exp)
        P.act(en[:].rearrange("p a n -> p (a n)"), bC[:, 0:256], AF.Exp, scale=-1.0)
        P.act(egl[:], bC[:, 256:258], AF.Exp)
        P.tt(qt[:], qT[:], eg[:], ALU.mult)
        P.tt(kt2[:], kT[:], en[:], ALU.mult, e='pool')
        P.tt(kdec[:], bB[:, 256:512], gcs[:], ALU.subtract)
        P.act(kdec[:], kdec[:], AF.Exp)
        P.tt(kdec[:], kdec[:], kv[:, 0:256], ALU.mult)
        for h in range(2):
            P.mm(bC[:, 384:512], kt2[:, h, :], qt[:, h, :], start=(h == 0), stop=(h == 1))
        P.tt(attm[:], bC[:, 384:512], inc[:], ALU.mult)
        P.mm(bA[:, 256:512], attm[:], kv[:, 256:512], start=True, stop=False)
        P.mm(bA[:, 256:512], qt[:, 0, :], S[:, 0, :], start=False, stop=False)
        P.mm(bA[:, 256:512], qt[:, 1, :], S[:, 1, :], start=False, stop=True)
        P.cp(o[:], bA[:, 256:512], e='act')
        for h in range(2):
            P.mm(bD[:, h * 256:(h + 1) * 256], kdec[:, h * 128:(h + 1) * 128], kv[:, 256:512])
        for h in range(2):
            P.stt(S[:, h, :], S[:, h, :], egl[:, h:h + 1], bD[:, h * 256:(h + 1) * 256], ALU.mult, ALU.add)
        P.dma((S_o if d == 0 else S_ob)[t], o[:])

    if 'C' in phases:
        BF = [mkbufs(0), mkbufs(1)]
        orders = [list(range(NT)), list(range(NCT - 1, -1, -1)) + list(range(NT - 1, NCT - 1, -1))]
        for i in range(NT):
            for d in range(2):
                chunk(d, orders[d][i], BF[d])
        of = P.sb([128, 256]); ob = P.sb([128, 256])
        for t in range(NT):
            P.dma(of[:], S_o[t]); P.dma(ob[:], S_ob[t])
            P.tt(of[:], of[:], ob[:], ALU.add)
            P.dma(out_o[t * 128:(t + 1) * 128, :], of[:])
    P.finish([out_o, out_sg])
    P.barrier()
    return P


def run_mixer_odd(x_all, modv, norm_g, w_in, wg2, bg, NT, NCT):
    P = two_pass(build_mixer_odd, NT, NCT)
    base = np_inputs(norm_g, modv[0, 1], modv[0, 0], modv[1, 1], modv[1, 0])
    maps = []
    for j in range(NCORES):
        h, s = divmod(j, 2)
        qc = np.arange(h * 256, (h + 1) * 256); kc_ = 1024 + qc
        vc = 2048 + h * 512 + s * 256 + np.arange(256); gc_ = 4096 + h * 512 + s * 256 + np.arange(256)
        w2 = np.stack([np.concatenate([wg2[d][:, h * 256:(h + 1) * 256], bg[d][None, h * 256:(h + 1) * 256]], axis=0) for d in range(2)])
        m = dict(base)
        m.update({"x_all": x_all,
                  "w_fm": np.ascontiguousarray(w_in[:, np.concatenate([qc, kc_])]),
                  "w_gd": np.ascontiguousarray(w_in[:, 6144:6176]),
                  "w_tm": np.ascontiguousarray(w_in[:, np.concatenate([kc_, vc, gc_])]),
                  "wg2": np.ascontiguousarray(w2.astype(np.float32))})
        maps.append(m)
    res = _run(P, maps)
    o = np.concatenate([r["out_o"] for r in res], axis=1)
    sg = np.concatenate([r["out_sg"] for r in res], axis=1)
    return o, sg


def build_post_odd(NTL):
    P = Prog(); nc = P.nc
    P.make_consts()
    ident = P.c['ident']
    NTOK = NTL * 128
    x_in = P.din("x_in", [NTOK, D]); o_in = P.din("o_in", [NTOK, D]); sg_in = P.din("sg_in", [NTOK, D])
    w_out = P.din("w_out", [D, D])
    bc_in = P.din("bc_in", [2, 128, D])
    rw_in = P.din("rw_in", [128, KC * 8]); rb_in = P.din("rb_in", [128, 8])
    x_mid = P.dout("x_mid", [NTOK, D]); H2T = P.dout("H2T", [KC, 128, NTOK], BF16); G_out = P.dout("G_out", [128, NTL * 8])
    X_s = P.dscr("X_s", [NTOK, D])
    npj = NormProj(P, 1, keep32=True)
    gtb = P.sb([128, D]); gng = P.sb([128, D]); rw = P.sb([128, KC, 8]); rb = P.sb([128, 8])
    P.dma(gtb[:], bc_in[0]); P.dma(gng[:], bc_in[1]); P.dma(rw[:].rearrange("p k n -> p (k n)"), rw_in[:, :]); P.dma(rb[:], rb_in[:, :])
    wo = load_w_bf16(P, w_out, D, 'w_out')
    ot = P.sb([128, D]); sgt = P.sb([128, D]); aT = P.sb([128, KC, 128], BF16); xm = P.sb([128, D])
    ss4 = P.sb([128, 4]); G_all = P.sb([128, NTL, 8]); lg = P.sb([128, 8]); m8 = P.sb([128, 8]); w12 = P.sb([128, 2])
    e1 = P.sb([128, 8]); e2 = P.sb([128, 8])
    py = [P.bank(4 + i) for i in range(4)]
    for t in range(NTL):
        rows = slice(t * 128, (t + 1) * 128)
        P.dma(ot[:], o_in[rows, :]); P.dma(sgt[:], sg_in[rows, :]); P.dma(npj.xt[:], x_in[rows, :])
        for h in range(4):
            P.act(npj.junk[:, h * 512:(h + 1) * 512], ot[:, h * 512:(h + 1) * 512], AF.Square, accum_out=ss4[:, h:h + 1])
        P.ts(ss4[:], ss4[:], 1.0 / 512, EPS, ALU.mult, ALU.add)
        P.act(ss4[:], ss4[:], AF.Sqrt)
        P.recip(ss4[:], ss4[:])
        for h in range(4):
            P.stt(ot[:, h * 512:(h + 1) * 512], ot[:, h * 512:(h + 1) * 512], ss4[:, h:h + 1], gng[:, h * 512:(h + 1) * 512], ALU.mult, ALU.mult)
        P.tt(ot[:], ot[:], sgt[:], ALU.mult, e='pool')
        for kc in range(KC):
            b, o = divmod(kc, 4)
            P.tr(npj.pT[b][:, o * 128:(o + 1) * 128], ot[:, kc * 128:(kc + 1) * 128], ident[:])
        for b in range(4):
            P.cp(aT[:, b * 4:(b + 1) * 4, :], npj.pT[b][:, :].rearrange("p (g n) -> p g n", g=4), e='act')
        for dq in range(4):
            for kc in range(KC):
                P.mm(py[dq][:, :], aT[:, kc, :], wo[:, kc, dq * 512:(dq + 1) * 512], start=(kc == 0), stop=(kc == KC - 1))
            cs = slice(dq * 512, (dq + 1) * 512)
            P.tt(xm[:, cs], py[dq][:, :], gtb[:, cs], ALU.mult)
            P.tt(xm[:, cs], xm[:, cs], npj.xt[:, cs], ALU.add)
        P.dma(X_s[rows, :], xm[:]); P.dma(x_mid[rows, :], xm[:])
        npj.tile(X_s[rows, :], 0)
        P.dma(H2T[:, :, rows].rearrange("k p n -> p k n"), npj.hT[:])
        for kc in range(KC):
            P.mm(py[0][:, 0:8], npj.hT32[:, kc, :], rw[:, kc, :], start=(kc == 0), stop=(kc == KC - 1))
        P.tt(lg[:], py[0][:, 0:8], rb[:], ALU.add)
        r_, w_ = P._rw([lg[:]], [m8[:]])
        P.op('dve', r_, w_, lambda: nc.vector.max(out=m8[:], in_=lg[:]))
        P.tt(w12[:, 0:1], m8[:, 0:1], m8[:, 1:2], ALU.subtract)
        P.act(w12[:, 0:1], w12[:, 0:1], AF.Sigmoid)
        P.ts(w12[:, 1:2], w12[:, 0:1], -1.0, 1.0, ALU.mult, ALU.add)
        P.ts(e1[:], lg[:], m8[:, 0:1], w12[:, 0:1], ALU.is_equal, ALU.mult)
        P.ts(e2[:], lg[:], m8[:, 1:2], w12[:, 1:2], ALU.is_equal, ALU.mult)
        P.tt(G_all[:, t, :], e1[:], e2[:], ALU.add)
    P.dma(G_out[:, :], G_all[:].rearrange("p t n -> p (t n)"))
    P.finish([x_mid, H2T, G_out])
    P.barrier()
    return P


def run_post_odd(x1, o, sg, modv, gla_norm_g, norm2_g, w_out, router_w, router_b):
    L = x1.shape[0]; lt = L // NCORES; NTL = lt // 128
    P = two_pass(build_post_odd, NTL)
    base = np_inputs(norm2_g, modv[0, 4], modv[0, 3])
    bc = np.ascontiguousarray(np.broadcast_to(np.stack([modv[0, 2], np.tile(gla_norm_g, 4)])[:, None, :], (2, 128, D)))
    rw = np.ascontiguousarray(router_w.reshape(KC, 128, 8).transpose(1, 0, 2).reshape(128, KC * 8))
    rb = np.ascontiguousarray(np.broadcast_to(router_b[None, :], (128, 8)))
    maps = []
    for j in range(NCORES):
        sl = slice(j * lt, (j + 1) * lt)
        m = dict(base)
        m.update({"x_in": np.ascontiguousarray(x1[sl]), "o_in": np.ascontiguousarray(o[sl]), "sg_in": np.ascontiguousarray(sg[sl]),
                  "w_out": w_out, "bc_in": bc, "rw_in": rw, "rb_in": rb})
        maps.append(m)
    res = _run(P, maps)
    x_mid = np.concatenate([r["x_mid"] for r in res], axis=0)
    H2T = np.concatenate([r["H2T"] for r in res], axis=2)
    G = np.concatenate([r["G_out"].reshape(128, NTL, 8).transpose(1, 0, 2).reshape(lt, 8) for r in res], axis=0)
    return x_mid, H2T, G


def build_expert(NTOK):
    P = Prog(); nc = P.nc
    NT = NTOK // 128
    H2T = P.din("H2T", [KC, 128, NTOK], BF16)
    gate = P.din("gate", [128, NT])
    w_gate = P.din("w_gate", [D, FFN_H]); w_up = P.din("w_up", [D, FFN_H]); w_down = P.din("w_down", [FFN_H, D])
    y = P.dout("y", [NTOK, D])
    Wg_s = P.dscr("Wg_s", [HB, 128, KC, 128], BF16); Wu_s = P.dscr("Wu_s", [HB, 128, KC, 128], BF16)
    Wd_s = P.dscr("Wd_s", [4, 128, HB, 512], BF16)
    gt = P.sb([128, NT])
    P.dma(gt[:], gate[:, :])
    ffn_cast_weights(P, w_gate, w_up, w_down, Wg_s, Wu_s, Wd_s)
    yo = [P.sb([128, 512]) for _ in range(2)]
    cnt = [0]

    def epi(row0, dq, ps):
        b = cnt[0] % 2; cnt[0] += 1
        t = row0 // 128
        P.act(yo[b][:], ps[:, :], AF.Identity, scale=gt[:, t:t + 1])
        P.dma(y[row0:row0 + 128, dq * 512:(dq + 1) * 512], yo[b][:])

    groups = [(s0, min(512, NTOK - s0)) for s0 in range(0, NTOK, 512)]
    ffn_groups(P, H2T, groups, Wg_s, Wu_s, Wd_s, epi)
    P.finish([y])
    P.barrier()
    return P


def run_experts(H2T, G, w_gate, w_up, w_down):
    L = G.shape[0]
    P = two_pass(build_expert, L)
    maps = []
    for e in range(NCORES):
        maps.append({"H2T": H2T, "gate": np.ascontiguousarray(G[:, e].reshape(L // 128, 128).T),
                     "w_gate": w_gate[e], "w_up": w_up[e], "w_down": w_down[e]})
    res = _run(P, maps)
    return [r["y"] for r in res]


def build_final(NTL):
    P = Prog(); nc = P.nc
    NTOK = NTL * 128
    x_mid = P.din("x_mid", [NTOK, D]); parts = P.din("parts", [NCORES, NTOK, D]); bc_in = P.din("bc_in", [2, 128, D])
    out = P.dout("out", [NTOK, D])
    gtb = P.sb([128, D]); fg = P.sb([128, D])
    P.dma(gtb[:], bc_in[0]); P.dma(fg[:], bc_in[1])
    xm = P.sb([128, D]); pt = [P.sb([128, D]) for _ in range(NCORES)]; junk = P.sb([128, D], BF16); ss = P.sb([128, 1])
    for t in range(NTL):
        rows = slice(t * 128, (t + 1) * 128)
        P.dma(xm[:], x_mid[rows, :])
        for e in range(NCORES):
            P.dma(pt[e][:], parts[e, rows, :])
        for (a, b, eng) in ((0, 1, 'dve'), (2, 3, 'pool'), (4, 5, 'dve'), (6, 7, 'pool'), (0, 2, 'dve'), (4, 6, 'pool'), (0, 4, 'dve')):
            P.tt(pt[a][:], pt[a][:], pt[b][:], ALU.add, e=eng)
        P.tt(pt[0][:], pt[0][:], gtb[:], ALU.mult)
        P.tt(xm[:], xm[:], pt[0][:], ALU.add, e='pool')
        P.act(junk[:], xm[:], AF.Square, accum_out=ss[:])
        P.ts(ss[:], ss[:], 1.0 / D, EPS, ALU.mult, ALU.add)
        P.act(ss[:], ss[:], AF.Sqrt)
        P.recip(ss[:], ss[:])
        P.stt(xm[:], xm[:], ss[:, 0:1], fg[:], ALU.mult, ALU.mult)
        P.dma(out[rows, :], xm[:])
    P.finish([out])
    P.barrier()
    return P


def run_final(x_mid, parts, gt2, final_g):
    L = x_mid.shape[0]; lt = L // NCORES; NTL = lt // 128
    P = two_pass(build_final, NTL)
    bc = np.ascontiguousarray(np.broadcast_to(np.stack([gt2, final_g])[:, None, :], (2, 128, D)))
    maps = []
    for j in range(NCORES):
        sl = slice(j * lt, (j + 1) * lt)
        maps.append({"x_mid": np.ascontiguousarray(x_mid[sl]), "parts": np.ascontiguousarray(np.stack([p[sl] for p in parts])), "bc_in": bc})
    res = _run(P, maps)
    return np.concatenate([r["out"] for r in res], axis=0)


def kernel(x, c, ctx, c_ctx, mod_w, mod_b, norm1_g, norm2_g, ev_w_in, ev_conv_qkv, ev_dn_a_log, ev_dn_dt_bias,
           ev_dn_norm_g, ev_lru_conv_w, ev_lru_conv_b, ev_lru_wa, ev_lru_ba, ev_lru_wx, ev_lru_bx, ev_lru_lambda,
           ev_w_out, ev_ffn_w_gate, ev_ffn_w_up, ev_ffn_w_down, od_w_in, od_gla_wg2, od_gla_bg, od_gla_norm_g,
           od_w_out, od_router_w, od_router_b, od_exp_w_gate, od_exp_w_up, od_exp_w_down, final_norm_g):
    f = lambda a: np.asarray(a, dtype=np.float32)
    x = f(x)[0]; ctx = f(ctx)[0]
    L = x.shape[0]; C = ctx.shape[0]
    NCT = C // 128; NT = NCT + L // 128
    modv = run_mod(f(c), f(c_ctx), f(mod_w), f(mod_b))
    x_all = np.concatenate([ctx, x], axis=0)
    act = run_mixer_even(x_all, modv[0], f(norm1_g)[0], f(ev_w_in)[0], f(ev_conv_qkv)[0], f(ev_dn_a_log)[0], f(ev_dn_dt_bias)[0],
                         f(ev_dn_norm_g)[0], f(ev_lru_conv_w)[0], f(ev_lru_conv_b)[0], f(ev_lru_wa)[0], f(ev_lru_ba)[0],
                         f(ev_lru_wx)[0], f(ev_lru_bx)[0], f(ev_lru_lambda)[0], NT, NCT)
    x1, c1 = run_post_even(x, ctx, act, modv[0], f(norm2_g)[0], f(ev_w_out)[0], f(ev_ffn_w_gate)[0], f(ev_ffn_w_up)[0], f(ev_ffn_w_down)[0])
    rows = L // GRID_W
    xr = x1.reshape(rows, GRID_W, D).swapaxes(0, 1).reshape(L, D)
    x_all = np.concatenate([c1, xr], axis=0)
    o, sg = run_mixer_odd(x_all, modv[1], f(norm1_g)[1], f(od_w_in)[0], f(od_gla_wg2)[0], f(od_gla_bg)[0], NT, NCT)
    unr = lambda a: a[C:].reshape(GRID_W, rows, D).swapaxes(0, 1).reshape(L, D)
    x_mid, H2T, G = run_post_odd(x1, unr(o), unr(sg), modv[1], f(od_gla_norm_g)[0], f(norm2_g)[1], f(od_w_out)[0],
                                 f(od_router_w)[0], f(od_router_b)[0])
    parts = run_experts(H2T, G, f(od_exp_w_gate)[0], f(od_exp_w_up)[0], f(od_exp_w_down)[0])
    out = run_final(x_mid, parts, modv[1][0, 5], f(final_norm_g))
    return out[None].astype(np.float32)
```
